# Optimizing a Trainium2 kernel written in Bass

```python
import jax, jax.numpy as jnp
from jax import lax
import numpy as np

D_MODEL = 1024
BATCH = 32
SEQ = 2048
DEPTH = 1

CHUNK = 64
Q_BLOCK = 128
FOX_HEADS = 8
FOX_HEAD_DIM = D_MODEL // 16
FOX_WIDTH = FOX_HEADS * FOX_HEAD_DIM
MLSTM_HEADS = 4
MLSTM_HEAD_DIM = D_MODEL // 8
MLSTM_WIDTH = MLSTM_HEADS * MLSTM_HEAD_DIM
CONV_WIDTH = 4
D_FF = 128 * ((8 * D_MODEL // 3 + 127) // 128)
RMS_EPS = 1e-6
IN_COLS = 3 * FOX_WIDTH + FOX_HEADS + 4 * MLSTM_WIDTH + 2 * MLSTM_HEADS + 2 * D_MODEL

kernel_name = 'fox_mlstm_macaron_gated_hybrid'


def _split_points():
    sizes = [FOX_WIDTH, FOX_WIDTH, FOX_WIDTH, FOX_HEADS,
             2 * MLSTM_WIDTH, MLSTM_WIDTH, MLSTM_HEADS, MLSTM_HEADS, MLSTM_WIDTH]
    pts, acc = [], 0
    for s in sizes:
        acc += s
        pts.append(acc)
    return pts


def rms_norm(x, g):
    xf = x.astype(jnp.float32)
    y = xf * lax.rsqrt(jnp.mean(xf * xf, axis=-1, keepdims=True) + RMS_EPS)
    return (y * g.astype(jnp.float32)).astype(x.dtype)


def swiglu_ffn(x, w_in, w_out):
    gate, up = jnp.split(x @ w_in, 2, axis=-1)
    return (jax.nn.silu(gate) * up) @ w_out


def causal_depthwise_conv(x, w, b):
    K, C = w.shape
    y = lax.conv_general_dilated(x, w[:, None, :].astype(x.dtype), window_strides=(1,),
                                 padding=[(K - 1, 0)], dimension_numbers=('NWC', 'WIO', 'NWC'),
                                 feature_group_count=C)
    return y + b.astype(x.dtype)


def forgetting_attention(q, k, v, log_f):
    B, S, H, Dh = q.shape
    c = jnp.cumsum(log_f, axis=1).transpose(0, 2, 1)
    scale = Dh ** -0.5
    outs = []
    for i in range(S // Q_BLOCK):
        q0, q1 = i * Q_BLOCK, (i + 1) * Q_BLOCK
        s = jnp.einsum('bqhd,bkhd->bhqk', q[:, q0:q1], k[:, :q1]) * scale
        s = s + c[:, :, q0:q1, None] - c[:, :, None, :q1]
        t_pos = jnp.arange(q0, q1)[:, None]
        s_pos = jnp.arange(q1)[None, :]
        s = jnp.where(s_pos <= t_pos, s, -jnp.inf)
        p = jax.nn.softmax(s, axis=-1)
        outs.append(jnp.einsum('bhqk,bkhd->bqhd', p, v[:, :q1]))
    return jnp.concatenate(outs, axis=1)


def mlstm_chunkwise(q, k, v, i_pre, log_f):
    B, S, H, Dk = q.shape
    Dv = v.shape[-1]
    nc = S // CHUNK

    def to_chunks(a):
        a = a.reshape((B, nc, CHUNK, H) + a.shape[3:])
        return jnp.moveaxis(a, (1, 3), (0, 2))

    xs = (to_chunks(q * Dk ** -0.5), to_chunks(k), to_chunks(v), to_chunks(i_pre), to_chunks(log_f))
    causal = jnp.tril(jnp.ones((CHUNK, CHUNK), dtype=bool))

    def step(carry, xs_c):
        C, n, m = carry
        qb, kb, vb, ib, fb = xs_c
        b = jnp.cumsum(fb, axis=-1)
        g = b[..., -1]
        dmat = jnp.where(causal, b[..., :, None] - b[..., None, :] + ib[..., None, :], -jnp.inf)
        m_inter = b + m[..., None]
        m_t = jnp.maximum(jnp.max(dmat, axis=-1), m_inter)
        s = jnp.einsum('bhld,bhsd->bhls', qb, kb) * jnp.exp(dmat - m_t[..., None])
        inter = jnp.exp(m_inter - m_t)
        num = jnp.einsum('bhls,bhse->bhle', s, vb) + inter[..., None] * jnp.einsum('bhld,bhde->bhle', qb, C)
        den = jnp.sum(s, axis=-1) + inter * jnp.einsum('bhld,bhd->bhl', qb, n)
        h = num / jnp.maximum(jnp.abs(den), jnp.exp(-m_t))[..., None]
        kdec = g[..., None] - b + ib
        m_new = jnp.maximum(g + m, jnp.max(kdec, axis=-1))
        wk = jnp.exp(kdec - m_new[..., None])
        carry_dec = jnp.exp(g + m - m_new)
        C_new = carry_dec[..., None, None] * C + jnp.einsum('bhs,bhsd,bhse->bhde', wk, kb, vb)
        n_new = carry_dec[..., None] * n + jnp.einsum('bhs,bhsd->bhd', wk, kb)
        return (C_new, n_new, m_new), h

    init = (jnp.zeros((B, H, Dk, Dv), jnp.float32), jnp.zeros((B, H, Dk), jnp.float32),
            jnp.zeros((B, H), jnp.float32))
    _, hs = lax.scan(step, init, xs)
    return jnp.moveaxis(hs, (0, 2), (1, 3)).reshape(B, S, H * Dv)


def hybrid_mixer(h, w_in, fox_f_bias, mlstm_conv_w, mlstm_conv_b, mlstm_i_bias, mlstm_f_bias,
                 w_up_fox, w_up_mlstm, w_out):
    B, S, _ = h.shape
    proj = (h @ w_in).astype(jnp.float32)
    fq, fk, fv, ff, mqk, mv, mi, mf, mo, gates = jnp.split(proj, _split_points(), axis=-1)
    fox_log_f = jax.nn.log_sigmoid(ff + fox_f_bias.astype(jnp.float32))
    a = forgetting_attention(fq.reshape(B, S, FOX_HEADS, FOX_HEAD_DIM),
                             fk.reshape(B, S, FOX_HEADS, FOX_HEAD_DIM),
                             fv.reshape(B, S, FOX_HEADS, FOX_HEAD_DIM), fox_log_f)
    a = a.reshape(B, S, FOX_WIDTH) @ w_up_fox.astype(jnp.float32)
    mqk = jax.nn.silu(causal_depthwise_conv(mqk, mlstm_conv_w.astype(jnp.float32), mlstm_conv_b))
    mq, mk = jnp.split(mqk, 2, axis=-1)
    hm = mlstm_chunkwise(mq.reshape(B, S, MLSTM_HEADS, MLSTM_HEAD_DIM),
                         mk.reshape(B, S, MLSTM_HEADS, MLSTM_HEAD_DIM),
                         mv.reshape(B, S, MLSTM_HEADS, MLSTM_HEAD_DIM),
                         mi + mlstm_i_bias.astype(jnp.float32),
                         jax.nn.log_sigmoid(mf + mlstm_f_bias.astype(jnp.float32)))
    bm = (jax.nn.sigmoid(mo) * hm) @ w_up_mlstm.astype(jnp.float32)
    g_a, g_b = jnp.split(gates, 2, axis=-1)
    merged = jax.nn.sigmoid(g_a) * a + jax.nn.sigmoid(g_b) * bm
    return (merged @ w_out.astype(jnp.float32)).astype(h.dtype)


def setup_inputs(seed: int = 0) -> dict:
    key = jax.random.key(seed)
    ks = jax.random.split(key, 20)
    L, D = DEPTH, D_MODEL

    def nrm(k, shape, fan_in):
        return jax.random.normal(k, shape, jnp.float32) * fan_in ** -0.5

    def gain(k, shape):
        return 1.0 + 0.05 * jax.random.normal(k, shape, jnp.float32)

    return {
        'x': jax.random.normal(ks[0], (BATCH, SEQ, D), jnp.float32),
        'ffn1_norm': gain(ks[1], (L, D)),
        'ffn1_w_in': nrm(ks[2], (L, D, 2 * D_FF), D),
        'ffn1_w_out': nrm(ks[3], (L, D_FF, D), D_FF),
        'mix_norm': gain(ks[4], (L, D)),
        'w_in': nrm(ks[5], (L, D, IN_COLS), D),
        'fox_f_bias': 1.0 + 0.1 * jax.random.normal(ks[6], (L, FOX_HEADS), jnp.float32),
        'mlstm_conv_w': nrm(ks[7], (L, CONV_WIDTH, 2 * MLSTM_WIDTH), CONV_WIDTH),
        'mlstm_conv_b': 0.02 * jax.random.normal(ks[8], (L, 2 * MLSTM_WIDTH), jnp.float32),
        'mlstm_i_bias': 0.1 * jax.random.normal(ks[9], (L, MLSTM_HEADS), jnp.float32),
        'mlstm_f_bias': jnp.linspace(3.0, 6.0, MLSTM_HEADS, dtype=jnp.float32)[None, :]
                        + 0.1 * jax.random.normal(ks[10], (L, MLSTM_HEADS), jnp.float32),
        'w_up_fox': nrm(ks[11], (L, FOX_WIDTH, D), FOX_WIDTH),
        'w_up_mlstm': nrm(ks[12], (L, MLSTM_WIDTH, D), MLSTM_WIDTH),
        'w_out': nrm(ks[13], (L, D, D), D),
        'ffn2_norm': gain(ks[14], (L, D)),
        'ffn2_w_in': nrm(ks[15], (L, D, 2 * D_FF), D),
        'ffn2_w_out': nrm(ks[16], (L, D_FF, D), D_FF),
        'final_norm': gain(ks[17], (D,)),
    }


def reference(x, ffn1_norm, ffn1_w_in, ffn1_w_out, mix_norm, w_in, fox_f_bias, mlstm_conv_w,
              mlstm_conv_b, mlstm_i_bias, mlstm_f_bias, w_up_fox, w_up_mlstm, w_out,
              ffn2_norm, ffn2_w_in, ffn2_w_out, final_norm):
    for l in range(DEPTH):
        x = x + 0.5 * swiglu_ffn(rms_norm(x, ffn1_norm[l]), ffn1_w_in[l], ffn1_w_out[l])
        x = x + hybrid_mixer(rms_norm(x, mix_norm[l]), w_in[l], fox_f_bias[l], mlstm_conv_w[l],
                             mlstm_conv_b[l], mlstm_i_bias[l], mlstm_f_bias[l],
                             w_up_fox[l], w_up_mlstm[l], w_out[l])
        x = x + 0.5 * swiglu_ffn(rms_norm(x, ffn2_norm[l]), ffn2_w_in[l], ffn2_w_out[l])
    return rms_norm(x, final_norm)
```

```python
import numpy as np
import concourse.bass as bass
import concourse.mybir as mybir
from concourse.bass_utils import run_bass_kernel_spmd

F32 = mybir.dt.float32
BF16 = mybir.dt.bfloat16
AF = mybir.ActivationFunctionType
ALU = mybir.AluOpType
AX = mybir.AxisListType

DT_SIZE = {F32: 4, BF16: 2}
PAGE = 512
SB0 = 16896
SB_SIZE = 229376 - SB0


class _Op:
    __slots__ = ("fn", "waits", "signal", "eng", "chan", "tag")

    def __init__(self, fn, eng, chan):
        self.fn = fn
        self.waits = {}
        self.signal = False
        self.eng = eng
        self.chan = chan


class Prog:
    COMPUTE = ("pe", "act", "dve", "pool")

    def __init__(self, nc):
        self.nc = nc
        self.ops = {e: [] for e in ("pe", "act", "dve", "pool", "sp")}
        self.base = {}
        self.pages = {}
        self.ident_ops = {}
        self.known = {}
        self.chan_consumed = {}
        self.sb_top = 0
        self.chan_list = []

    def sb(self, name, shape, dtype, addr):
        size = int(np.prod(shape[1:])) * DT_SIZE[dtype]
        assert addr % 32 == 0, (name, addr)
        assert addr + size <= SB_SIZE, (name, addr, size)
        addr = addr + SB0
        t = self.nc.alloc_sbuf_tensor_at(name, list(shape), dtype, offset=addr)
        self.base[t.name] = ("sb", addr)
        return t

    def reg_psum(self, t):
        self.base[t.name] = ("ps", 0)

    def reg_dram(self, ap, name=None):
        self.base[ap.tensor.name] = ("d:" + ap.tensor.name, 0)

    def region(self, ap):
        tn = ap.tensor.name
        space, base = self.base[tn]
        dims = ap.ap
        esz = DT_SIZE.get(ap.dtype, 4)
        off = int(ap.offset)
        if space.startswith("d:"):
            ext = 1
            for st, cnt in dims:
                ext += (cnt - 1) * abs(st)
            return (space, off * esz, (off + ext) * esz, 0, 1)
        pstep = dims[0][0]
        if pstep == 0:
            pstep = 1 << 40
        plo = off // pstep
        flo = off % pstep
        ext = 1
        for st, cnt in dims[1:]:
            ext += (cnt - 1) * abs(st)
        lo = base + flo * esz
        hi = base + (flo + ext) * esz
        phi = plo + dims[0][1]
        if space == "ps":
            lo = (lo // 2048) * 2048
            hi = ((hi + 2047) // 2048) * 2048
            plo = (plo // 32) * 32
            phi = ((phi + 31) // 32) * 32
        return (space, lo, hi, plo, phi)

    def _ident_idx(self, op_eng, chan):
        ident = chan if chan is not None else op_eng
        lst = self.ident_ops.setdefault(ident, [])
        return ident, lst

    def _add_dep(self, op, ident, idx):
        if ident.startswith("dma:"):
            idx = len(self.ident_ops[ident])
            if self.chan_consumed.get(ident, 0) < idx:
                self.chan_consumed[ident] = idx
        kn = self.known.setdefault(op.eng, {})
        if kn.get(ident, 0) >= idx:
            return
        if op.waits.get(ident, 0) < idx:
            op.waits[ident] = idx

    def add(self, eng, fn, reads=(), writes=(), chan=None):
        op = _Op(fn, eng, chan)
        op.tag = (writes[0].tensor.name if writes else "", len(self.ops[eng]))
        ident, lst = self._ident_idx(eng, chan)
        my_idx = len(lst) + 1
        racc = [self.region(a) for a in reads]
        wacc = [self.region(a) for a in writes]
        wacc += [a for a in racc if a[0] == "ps"]
        racc = [a for a in racc if a[0] != "ps"]
        same_ok_all = (ident == "pe")
        if chan is not None:
            k = self.chan_consumed.get(ident, 0)
            if k > 0:
                kn0 = self.known.setdefault(eng, {})
                if kn0.get(ident, 0) < k:
                    op.waits[ident] = k
        for kindnew, accs in (("R", racc), ("W", wacc)):
            for (space, lo, hi, plo, phi) in accs:
                for pg in range(lo // PAGE, (hi - 1) // PAGE + 1):
                    recs = self.pages.get((space, pg))
                    if not recs:
                        continue
                    for (rid, rkind), rec in recs.items():
                        if kindnew == "R" and rkind == "R":
                            continue
                        ridx, rlo, rhi, rplo, rphi = rec
                        if rlo >= hi or rhi <= lo or rplo >= phi or rphi <= plo:
                            continue
                        if rid == ident:
                            if same_ok_all:
                                continue
                            if ident.startswith("dma:"):
                                if rkind == "W" and kindnew == "W":
                                    continue
                            else:
                                if not (rkind == "W" and kindnew == "R"):
                                    continue
                        self._add_dep(op, rid, ridx)
        for kindnew, accs in (("R", racc), ("W", wacc)):
            for (space, lo, hi, plo, phi) in accs:
                for pg in range(lo // PAGE, (hi - 1) // PAGE + 1):
                    recs = self.pages.setdefault((space, pg), {})
                    pglo, pghi = pg * PAGE, (pg + 1) * PAGE
                    clo, chi = max(lo, pglo), min(hi, pghi)
                    if kindnew == "W":
                        for key in [k for k, r in recs.items()
                                    if max(r[1], pglo) >= clo and min(r[2], pghi) <= chi
                                    and r[3] >= plo and r[4] <= phi]:
                            del recs[key]
                    key = (ident, kindnew)
                    r = recs.get(key)
                    if r is None:
                        recs[key] = [my_idx, clo, chi, plo, phi]
                    else:
                        r[0] = my_idx
                        r[1] = min(r[1], clo); r[2] = max(r[2], chi)
                        r[3] = min(r[3], plo); r[4] = max(r[4], phi)
        kn = self.known.setdefault(eng, {})
        for wid, widx in op.waits.items():
            kn[wid] = max(kn.get(wid, 0), widx)
            tgt = self.ident_ops[wid][widx - 1]
            tgt.signal = True
        lst.append(op)
        self.ops[eng].append(op)
        return op

    def emit(self, final_waits=()):
        nc = self.nc
        idents = list(self.ident_ops.keys())
        from contextlib import ExitStack
        with ExitStack() as es:
            sems = {}
            for ident in idents:
                sems[ident] = es.enter_context(nc.semaphore("s_" + ident.replace(":", "_")))
            tick = {}
            for ident in idents:
                cnt = 0
                tl = []
                isdma = ident.startswith("dma:")
                for op in self.ident_ops[ident]:
                    if isdma:
                        cnt += 16
                    elif op.signal:
                        cnt += 1
                    tl.append(cnt)
                tick[ident] = tl
                assert cnt < 60000, (ident, cnt)
            block = es.enter_context(nc.Block())

            def run(engname, e):
                for op in self.ops[engname]:
                    for wid, widx in op.waits.items():
                        e.wait_ge(sems[wid], tick[wid][widx - 1])
                    ins = op.fn(e)
                    if op.chan is not None:
                        ins.then_inc(sems[op.chan], 16)
                    elif op.signal:
                        ins.then_inc(sems[op.eng], 1)

            @block.tensor
            def _(e):
                run("pe", e)

            @block.scalar
            def _(e):
                run("act", e)

            @block.vector
            def _(e):
                run("dve", e)

            @block.gpsimd
            def _(e):
                run("pool", e)

            @block.sync
            def _(e):
                run("sp", e)
                for ident in idents:
                    if tick[ident] and tick[ident][-1] > 0 and (ident.startswith("dma:")):
                        e.wait_ge(sems[ident], tick[ident][-1])

    def dma(self, out, in_, chan, q="sp"):
        ch = "dma:" + chan
        return self.add(q, lambda e: e.dma_start(out=out, in_=in_), reads=[in_], writes=[out], chan=ch)

    def mm(self, out, lhsT, rhs, start=True, stop=True, **kw):
        rd = [lhsT, rhs] + ([] if start else [out])
        return self.add("pe", lambda e: e.matmul(out, lhsT, rhs, start=start, stop=stop, **kw),
                        reads=rd, writes=[out])

    def transpose(self, out, in_, ident):
        return self.add("pe", lambda e: e.transpose(out, in_, ident), reads=[in_, ident], writes=[out])

    def act(self, out, in_, func, bias=None, scale=None, accum_out=None, eng="act"):
        rd = [in_]
        kw = {}
        if bias is not None:
            kw["bias"] = bias
            if not isinstance(bias, (int, float)):
                rd.append(bias)
        if scale is not None:
            kw["scale"] = scale
            if not isinstance(scale, (int, float)):
                rd.append(scale)
        wr = [out]
        if accum_out is not None:
            kw["accum_out"] = accum_out
            wr.append(accum_out)
        return self.add("act", lambda e: e.activation(out, in_, func, **kw), reads=rd, writes=wr)

    def tt(self, out, in0, in1, op, eng="dve"):
        return self.add(eng, lambda e: e.tensor_tensor(out, in0, in1, op), reads=[in0, in1], writes=[out])

    def ts(self, out, in0, s1, s2, op0, op1=None, eng="dve", accum_out=None):
        rd = [in0] + [s for s in (s1, s2) if s is not None and not isinstance(s, (int, float))]
        wr = [out] + ([accum_out] if accum_out is not None else [])
        if op1 is None:
            return self.add(eng, lambda e: e.tensor_single_scalar(out, in0, s1, op0), reads=rd, writes=wr)
        kw = {}
        if accum_out is not None:
            kw["accum_out"] = accum_out
        return self.add(eng, lambda e: e.tensor_scalar(out, in0, s1, s2, op0, op1, **kw), reads=rd, writes=wr)

    def stt(self, out, in0, scalar, in1, op0, op1, eng="dve"):
        rd = [in0, in1] + ([] if isinstance(scalar, (int, float)) else [scalar])
        return self.add(eng, lambda e: e.scalar_tensor_tensor(out, in0, scalar, in1, op0, op1),
                        reads=rd, writes=[out])

    def copy(self, out, in_, eng="dve"):
        if eng == "act":
            return self.add("act", lambda e: e.copy(out, in_), reads=[in_], writes=[out])
        return self.add(eng, lambda e: e.tensor_copy(out, in_), reads=[in_], writes=[out])

    def memset(self, out, val, eng="pool"):
        return self.add(eng, lambda e: e.memset(out, val), reads=[], writes=[out])

    def recip(self, out, in_):
        return self.add("dve", lambda e: e.reciprocal(out, in_), reads=[in_], writes=[out])

    def scan(self, out, d0, d1, init, op0, op1):
        rd = [d0, d1] + ([] if isinstance(init, (int, float)) else [init])
        return self.add("dve", lambda e: e.tensor_tensor_scan(out, d0, d1, init, op0, op1),
                        reads=rd, writes=[out])

    def affine_select(self, out, in_, pattern, cmp, fill, base, cm):
        return self.add("pool", lambda e: e.affine_select(out, in_, pattern, cmp, fill, base=base,
                                                          channel_multiplier=cm),
                        reads=[in_], writes=[out])


D = 1024
SEQ = 2048
NT = SEQ // 128
DFF = 2816
NJ = DFF // 128
KC = D // 128
FH, FD = 8, 64
MH, MD = 4, 128
INC = 5648
C_FQ, C_FK, C_FV, C_FF = 0, 512, 1024, 1536
C_MQK, C_MV, C_MI, C_MF, C_MO, C_G = 1544, 2568, 3080, 3084, 3088, 3600
EPS = 1e-6
KB = 1024


class Stream:
    def __init__(self):
        self.items = []
        self.issued = 0

    def push(self, thunk, major=True):
        self.items.append((thunk, major))
        return len(self.items) - 1

    def ensure(self, k):
        k = min(k, len(self.items) - 1)
        while self.issued <= k:
            self.items[self.issued][0]()
            self.issued += 1

    def prefetch_after(self, k, limit):
        j = k + 1
        while j < len(self.items) and not self.items[j][1]:
            j += 1
        self.ensure(min(j, limit))


def build_program(n_seq, do_ffn1=True, do_mix=True, do_ffn2=True, dbg=None):
    nc = bass.Bass("TRN2", target_bir_lowering=False)
    P = Prog(nc)

    def din(name, shape):
        a = nc.dram_tensor(name, list(shape), F32, kind="ExternalInput").ap()
        P.reg_dram(a)
        return a

    x_d = din("x", [n_seq * SEQ, D])
    w1i_d = din("ffn1_w_in", [D, 2 * DFF])
    w1o_d = din("ffn1_w_out", [DFF, D])
    w2i_d = din("ffn2_w_in", [D, 2 * DFF])
    w2o_d = din("ffn2_w_out", [DFF, D])
    win_d = din("w_in", [D, INC])
    wuf_d = din("w_up_fox", [512, D])
    wum_d = din("w_up_mlstm", [512, D])
    wo_d = din("w_out", [D, D])
    gfm_d = din("gains_fm", [128, 3 * KC])
    gfin_d = din("final_norm", [1, D])
    convw_d = din("conv_fm", [128, KC * 5])
    bias_d = din("biases", [1, 16])
    fbc_d = din("fbias_col", [8, 1])
    out_d = nc.dram_tensor("out", [n_seq * SEQ, D], F32, kind="ExternalOutput").ap()
    P.reg_dram(out_d)

    ps = nc.alloc_psum_tensor("ps", [128, 4096], F32)
    P.reg_psum(ps)

    def bank(b, n=512, off=0):
        return ps[:, b * 512 + off: b * 512 + off + n]

    X = P.sb("X", [128, NT, D], F32, 0)
    MISC = 184 * KB
    XS = [P.sb("XS%d" % i, [128, D], F32, MISC + i * 4 * KB) for i in range(2)]
    SG = [P.sb("SG%d" % i, [128, 512], F32, MISC + 8 * KB + i * 2 * KB) for i in range(2)]
    GF = P.sb("GF", [128, D], F32, MISC + 12 * KB)
    JUNK = P.sb("JUNK", [128, D], BF16, MISC + 16 * KB)
    IDENT = P.sb("IDENT", [128, 128], F32, MISC + 18 * KB)
    IDENTB = P.sb("IDENTB", [128, 128], BF16, MISC + 18 * KB + 512)
    SM = MISC + 19 * KB
    GFM = P.sb("GFM", [128, 3 * KC], F32, SM)
    STAT = P.sb("STAT", [128, 64], F32, SM + 128)
    CONVW = P.sb("CONVW", [128, KC * 5], F32, SM + 384)
    BIAS = P.sb("BIAS", [128, 16], F32, SM + 544)
    NBIAS = P.sb("NBIAS", [128, 16], F32, SM + 608)
    ONESB = P.sb("ONESB", [128, 128], BF16, SM + 704)
    XNTH = P.sb("XNTH", [128, KC, 1024], BF16, 64 * KB)
    AT = P.sb("AT", [128, NJ, 1024], BF16, 80 * KB)
    WOUT = P.sb("WOUT", [128, NJ, D], BF16, 124 * KB)
    WBLK = [(P.sb("WG%d" % i, [128, KC, 256], BF16, 168 * KB + i * 8 * KB),
             P.sb("WU%d" % i, [128, KC, 256], BF16, 172 * KB + i * 8 * KB)) for i in range(2)]

    P.dma(GFM[:], gfm_d[:, :], "c0")
    P.dma(CONVW[:], convw_d[:, :], "c0")
    P.dma(BIAS[:], bias_d[0:1, :].partition_broadcast(128), "c0")
    P.memset(IDENT[:], 1.0)
    P.affine_select(IDENT[:], IDENT[:], [[1, 128]], ALU.is_equal, 0.0, 0, -1)
    P.copy(IDENTB[:], IDENT[:], eng="pool")
    P.memset(ONESB[:], 1.0)
    P.ts(NBIAS[:], BIAS[:], -1.0, None, ALU.mult)

    def rms_rstd(tiles, col0):
        n = len(tiles)
        for i, t in enumerate(tiles):
            P.act(JUNK[:], X[:, t, :], AF.Square, accum_out=STAT[:, col0 + i: col0 + i + 1])
        sl = STAT[:, col0: col0 + n]
        P.ts(sl, sl, 1.0 / D, EPS, ALU.mult, ALU.add)
        P.act(sl, sl, AF.Sqrt)
        P.recip(sl, sl)

    def norm_transpose(t, rcol, gidx, dst, dcol, nbuf):
        xs = XS[nbuf % 2]
        rs = STAT[:, rcol: rcol + 1]
        P.add("act", lambda e: e.mul(xs[:], X[:, t, :], rs), reads=[X[:, t, :], rs], writes=[xs[:]])
        pb = 0 if nbuf % 2 == 0 else 2 * 512
        for c in range(KC):
            P.transpose(ps[:, pb + c * 128: pb + (c + 1) * 128], xs[:, c * 128:(c + 1) * 128], IDENT[:])
        g = GFM[:, gidx * KC:(gidx + 1) * KC].unsqueeze(2).to_broadcast([128, KC, 128])
        P.tt(dst[:, :, dcol: dcol + 128], ps[:, pb: pb + 1024].rearrange("p (c l) -> p c l", l=128), g, ALU.mult)

    pool_stream = Stream()

    def ffn_prepare(w_in_d, w_out_d):
        w_in_v = w_in_d.rearrange("(kc p) n -> p kc n", p=128)
        w_out_v = w_out_d.rearrange("(j p) n -> p j n", p=128)
        idx = {}

        def mk_blk(jb, slot_i):
            def th():
                wg, wu = WBLK[slot_i]
                P.dma(wg[:], w_in_v[:, :, jb * 256:(jb + 1) * 256], "wblk%d" % slot_i, q="pool")
                P.dma(wu[:], w_in_v[:, :, DFF + jb * 256: DFF + (jb + 1) * 256], "wblk%d" % slot_i, q="pool")
            return th

        def mk_wout(j0, j1):
            def th():
                P.dma(WOUT[:, j0:j1, :], w_out_v[:, j0:j1, :], "wout", q="pool")
            return th

        k = 0
        for hf in range(2):
            for jb in range(NJ // 2):
                idx[("blk", hf, jb)] = pool_stream.push(mk_blk(jb, k % 2))
                k += 1
                if hf == 0 and jb in (2, 4, 6, 8):
                    q = (jb // 2 - 1)
                    j0, j1 = [(0, 6), (6, 12), (12, 17), (17, 22)][q]
                    idx[("wout", q)] = pool_stream.push(mk_wout(j0, j1), major=False)
        return idx

    def final_norm_tile(s, t):
        sl = STAT[:, 16 + t: 17 + t]
        P.act(JUNK[:], X[:, t, :], AF.Square, accum_out=sl)
        P.ts(sl, sl, 1.0 / D, EPS, ALU.mult, ALU.add)
        P.act(sl, sl, AF.Sqrt)
        P.recip(sl, sl)
        ob = XS[t % 2]
        P.stt(ob[:], X[:, t, :], sl, GF[:], ALU.mult, ALU.mult)
        P.dma(out_v[s, :, t, :], ob[:], "xout")

    def ffn_phase(s, gidx, idx, final=False, nxt=None):
        slot = 0
        if final:
            P.dma(GF[:], gfin_d[0:1, :].partition_broadcast(128), "c0")
        rms_rstd(list(range(8)), 0)
        for i in range(8):
            norm_transpose(i, i, gidx, XNTH, i * 128, i)
        for hf in range(2):
            for jb in range(NJ // 2):
                cur = idx[("blk", hf, jb)]
                pool_stream.ensure(cur)
                pool_stream.prefetch_after(cur, idx[("blk", 1, NJ // 2 - 1)])
                wg, wu = WBLK[slot % 2]
                slot += 1
                for jj in range(2):
                    j = 2 * jb + jj
                    for nt in range(2):
                        pb = 2 + 2 * ((j * 2 + nt) % 2)
                        gb, ub = bank(pb), bank(pb + 1)
                        for kc in range(KC):
                            P.mm(gb, wg[:, kc, jj * 128:(jj + 1) * 128], XNTH[:, kc, nt * 512:(nt + 1) * 512],
                                 start=(kc == 0), stop=(kc == KC - 1))
                        for kc in range(KC):
                            P.mm(ub, wu[:, kc, jj * 128:(jj + 1) * 128], XNTH[:, kc, nt * 512:(nt + 1) * 512],
                                 start=(kc == 0), stop=(kc == KC - 1))
                        sg = SG[(j * 2 + nt) % 2]
                        P.act(sg[:], gb, AF.Silu)
                        P.tt(AT[:, j, nt * 512:(nt + 1) * 512], sg[:], ub, ALU.mult)
                if final and hf == 1 and nxt is not None and jb == 2:
                    for t in range(8):
                        P.dma(X[:, t, :], x_v[nxt, :, t, :], "xinA")
            pool_stream.ensure(idx[("wout", 3)])
            if hf == 0:
                rms_rstd(list(range(8, 16)), 8)
            for tt_ in range(8):
                t = hf * 8 + tt_
                for dh in range(2):
                    yb = bank(6 + (tt_ * 2 + dh) % 2)
                    for j in range(NJ):
                        P.mm(yb, AT[:, j, tt_ * 128:(tt_ + 1) * 128], WOUT[:, j, dh * 512:(dh + 1) * 512],
                             start=(j == 0), stop=(j == NJ - 1))
                    xsl = X[:, t, dh * 512:(dh + 1) * 512]
                    P.stt(xsl, yb, 0.5, xsl, ALU.mult, ALU.add)
                if hf == 0:
                    norm_transpose(8 + tt_, 8 + tt_, gidx, XNTH, tt_ * 128, tt_)
                if final:
                    final_norm_tile(s, t)

    import math
    XNT = P.sb("XNT", [128, KC, SEQ], BF16, 64 * KB)
    VF = P.sb("VF", [128, NT, 512], BF16, 96 * KB)
    AOT = P.sb("AOT", [128, 4, SEQ], BF16, 112 * KB)
    QE = [P.sb("QE%d" % i, [128, SEQ], BF16, (128 * KB, 184 * KB)[i]) for i in range(2)]
    KE = [P.sb("KE%d" % i, [128, SEQ], BF16, (132 * KB, 188 * KB)[i]) for i in range(2)]
    QO = [P.sb("QO%d" % i, [128, SEQ], BF16, (136 * KB, 192 * KB)[i]) for i in range(2)]
    KO = [P.sb("KO%d" % i, [128, SEQ], BF16, (140 * KB, 158 * KB)[i]) for i in range(2)]
    WV = P.sb("WV", [128, KC, 512], BF16, 128 * KB)
    PT = [P.sb("PT%d" % i, [128, 512], BF16, 144 * KB + i * KB) for i in range(4)]
    CF = [P.sb("CF%d" % i, [8, SEQ], F32, 148 * KB + i * 8 * KB) for i in range(2)]
    CS = [P.sb("CS%d" % i, [8, SEQ], BF16, 164 * KB + i * 4 * KB) for i in range(3)]
    WQK = [(P.sb("WQ4_0", [128, KC, 256], BF16, 176 * KB), P.sb("WK4_0", [128, KC, 256], BF16, 180 * KB)),
           (P.sb("WQ4_1", [128, KC, 256], BF16, 148 * KB), P.sb("WK4_1", [128, KC, 256], BF16, 152 * KB))]
    RD = P.sb("RD", [128, 512], F32, 156 * KB)
    RD2 = P.sb("RD2", [128, 512], F32, 158 * KB)
    VF2A = P.sb("VF2A", [128, 8, FH, 128], BF16, 96 * KB)
    VF2B = P.sb("VF2B", [128, 8, FH, 128], BF16, 184 * KB)
    QTP = P.sb("QTP", [128, MH, SEQ], BF16, 96 * KB)
    KTP = P.sb("KTP", [128, MH, SEQ], BF16, 128 * KB)
    VM = P.sb("VM", [128, NT, 512], BF16, 144 * KB)
    SO = P.sb("SO", [128, MH, SEQ], BF16, 160 * KB)
    WB = [P.sb("WB%d" % i, [128, KC, 128], BF16, 176 * KB + i * 2 * KB) for i in range(2)]
    WR = [P.sb("WR%d" % i, [128, KC, 128], BF16, 180 * KB + i * 2 * KB) for i in range(2)]
    WMV = P.sb("WMV", [128, KC, 512], BF16, 176 * KB)
    T0 = P.sb("T0", [128, 512], F32, 184 * KB)
    T1 = P.sb("T1", [128, 512], F32, 186 * KB)
    T0B = P.sb("T0B", [128, 512], F32, 188 * KB)
    T1B = P.sb("T1B", [128, 512], F32, 190 * KB)
    PCQ = P.sb("PCQ", [128, 520], F32, 192 * KB)
    CVQ = P.sb("CVQ", [128, 512], F32, 192 * KB + 2560)
    PCK = P.sb("PCK", [128, 520], F32, 192 * KB + 4608)
    CVK = P.sb("CVK", [128, 512], F32, 192 * KB + 7168)
    C32 = P.sb("C32", [128, MH, 256], F32, 176 * KB)
    CB = P.sb("CB", [128, MH, 256], BF16, 180 * KB)
    KTOK = P.sb("KTOK", [128, MH, 128], BF16, 182 * KB)
    STM = P.sb("STM", [128, MH, 128], BF16, 183 * KB)
    KTOK2 = P.sb("KTOK2", [128, MH, 128], BF16, 188 * KB)
    STM2 = P.sb("STM2", [128, MH, 128], BF16, 189 * KB)
    TC = P.sb("TC", [128, 512], F32, 190 * KB)
    TD = P.sb("TD", [128, 512], F32, 192 * KB)
    WO = P.sb("WO", [128, KC, D], BF16, 96 * KB)
    MT = P.sb("MT", [128, KC, SEQ], BF16, 128 * KB)
    WMG = [dict(uf=P.sb("WUF%d" % i, [128, 4, 128], BF16, 176 * KB + i * 6 * KB),
                um=P.sb("WUM%d" % i, [128, 4, 128], BF16, 177 * KB + i * 6 * KB),
                ga=P.sb("WGA%d" % i, [128, KC, 128], BF16, 178 * KB + i * 6 * KB),
                gb=P.sb("WGB%d" % i, [128, KC, 128], BF16, 180 * KB + i * 6 * KB)) for i in range(2)]
    SGA = P.sb("SGA", [128, 512], F32, 188 * KB)
    SGB = P.sb("SGB", [128, 512], F32, 190 * KB)
    M1 = P.sb("M1", [128, 512], F32, 192 * KB)
    M2 = P.sb("M2", [128, 512], F32, 194 * KB)
    C2 = 204 * KB
    MASKR = P.sb("MASKR", [128, 512], F32, C2)
    MASKNEG = P.sb("MASKNEG", [128, 128], F32, C2 + 2048)
    MASKU = P.sb("MASKU", [128, 128], BF16, C2 + 2560)
    GW32 = P.sb("GW32", [128, KC, 8], F32, C2 + 2816)
    EG = P.sb("EG", [128, MH, NT], F32, C2 + 3072)
    HIST = P.sb("HIST", [128, KC, 4], F32, C2 + 3328)
    CONSTS = P.sb("CONSTS", [128, 8], F32, C2 + 3456)
    FBC = P.sb("FBC", [8, 2], F32, MISC + 18 * KB + 896)
    WFG = P.sb("WFG2", [128, KC, 8], BF16, MISC + 18 * KB + 768)

    P.memset(MASKR[:], 1.0)
    P.memset(MASKR[:].rearrange("p (c l) -> p c l", l=128)[:, :, 0:1], 0.0)
    P.memset(MASKNEG[:], 0.0)
    P.affine_select(MASKNEG[:], MASKNEG[:], [[1, 128]], ALU.is_ge, -30000.0, 0, -1)
    P.memset(MASKU[:], 1.0)
    P.affine_select(MASKU[:], MASKU[:], [[1, 128]], ALU.is_ge, 0.0, 0, -1)
    P.memset(CONSTS[:, 0:1], math.log(MD ** -0.5))
    P.memset(CONSTS[:, 1:2], 1.0)
    P.dma(FBC[:, 0:1], fbc_d[:, :], "c0")
    P.ts(FBC[:, 1:2], FBC[:, 0:1], -1.0, None, ALU.mult)

    win_v = win_d.rearrange("(kc p) n -> p kc n", p=128)

    def wload(dst, col0, ncol, chan):
        P.dma(dst, win_v[:, :, col0: col0 + ncol], chan, q="pool")

    def mix_prepare():
        idx = {}
        S_ = pool_stream
        idx["wv"] = S_.push(lambda: (wload(WV[:], C_FV, 512, "mwa"), wload(WFG[:], C_FF, 8, "mwa")))
        for g in range(2):
            idx[("wqk", g)] = S_.push((lambda g=g: (wload(WQK[g][0][:], C_FQ + g * 256, 256, "wqk%d" % g),
                                                    wload(WQK[g][1][:], C_FK + g * 256, 256, "wqk%d" % g))))
        idx["wmv"] = S_.push(lambda: wload(WMV[:], C_MV, 512, "mwa"))
        k = 0
        for c in range(4):
            idx[("wo_", c)] = S_.push((lambda c=c, k=k: wload(WB[k % 2][:], C_MO + c * 128, 128, "wb%d" % (k % 2))))
            k += 1
        for h in range(MH):
            for qk in range(2):
                idx[("wqkm", h, qk)] = S_.push((lambda h=h, qk=qk, k=k: wload(WB[k % 2][:], C_MQK + (qk * 4 + h) * 128,
                                                                             128, "wb%d" % (k % 2))))
                k += 1
        wuf_v = wuf_d.rearrange("(kc p) n -> p kc n", p=128)
        wum_v = wum_d.rearrange("(kc p) n -> p kc n", p=128)
        wo_v = wo_d.rearrange("(kc p) n -> p kc n", p=128)
        for c in range(KC):
            def th(c=c):
                w = WMG[c % 2]
                ch = "wmg%d" % (c % 2)
                P.dma(w["uf"][:], wuf_v[:, :, c * 128:(c + 1) * 128], ch, q="pool")
                P.dma(w["um"][:], wum_v[:, :, c * 128:(c + 1) * 128], ch, q="pool")
                wload(w["ga"][:], C_G + c * 128, 128, ch)
                wload(w["gb"][:], C_G + D + c * 128, 128, ch)
            idx[("wmg", c)] = S_.push(th)
            if c == 1:
                idx["wo"] = S_.push(lambda: P.dma(WO[:], wo_v[:, :, :], "mwa", q="pool"), major=False)
        return idx

    def evac(dst, src, k, scale=None):
        if scale is None:
            P.copy(dst, src, eng=("act" if k % 2 == 0 else "dve"))
        elif k % 2 == 0:
            P.add("act", lambda e: e.mul(dst, src, scale), reads=[src], writes=[dst])
        else:
            P.ts(dst, src, scale, None, ALU.mult)

    dbg_outs = {}

    def dump(name, t):
        if not dbg or name not in dbg:
            return
        shape = list(t.shape)
        o = nc.dram_tensor("dbg_" + name, shape, t.dtype, kind="ExternalOutput").ap()
        P.reg_dram(o)
        P.dma(o, t[:], "dbg")

    def mixer_phase(s, idx):
        S_ = pool_stream
        S_.ensure(idx["wv"])
        rms_rstd(list(range(NT)), 32)
        for t in range(NT):
            norm_transpose(t, 32 + t, 1, XNT, t * 128, t)
        S_.ensure(idx[("wqk", 0)])
        for nt in range(4):
            b = bank(2 + nt % 2)
            for kc in range(KC):
                P.mm(b[0:8, :], WFG[:, kc, 0:8], XNT[:, kc, nt * 512:(nt + 1) * 512], start=(kc == 0), stop=(kc == KC - 1))
            P.act(CF[0][:, nt * 512:(nt + 1) * 512], b[0:8, :], AF.Exp, bias=FBC[:, 1:2], scale=-1.0)
        P.act(CF[0][:], CF[0][:], AF.Ln, bias=CONSTS[0:8, 1:2])
        P.scan(CF[1][:], CONSTS[0:8, 1:2].to_broadcast([8, SEQ]), CF[0][:], 0.0, ALU.mult, ALU.add)
        P.copy(CS[0][:], CF[1][:])
        P.tt(CF[0][:], CF[1][:], CS[0][:], ALU.subtract)
        P.copy(CS[1][:], CF[0][:])
        P.tt(CF[1][:], CF[0][:], CS[1][:], ALU.subtract)
        P.copy(CS[2][:], CF[1][:])
        P.memset(VF2A[:], 1.0)
        P.memset(VF2B[:], 1.0)
        for t in range(NT):
            b = bank(t % 2)
            for kc in range(KC):
                P.mm(b, XNT[:, kc, t * 128:(t + 1) * 128], WV[:, kc, :], start=(kc == 0), stop=(kc == KC - 1))
            vf = (VF2A if t < 8 else VF2B)[:, t % 8]
            bv = b.rearrange("p (h d) -> p h d", d=FD)
            evac(vf[:, 0::2, 0:64], bv[:, 0::2, :], 0)
            evac(vf[:, 1::2, 64:128], bv[:, 1::2, :], 1)
        for i in range(1):
            P.memset(QE[i][64:70, :], 1.0)
            P.memset(KE[i][64:70, :], -1.0)
            P.memset(QO[i][0:64, :], 0.0)
            P.memset(KO[i][0:64, :], 0.0)
            P.memset(QO[i][0:6, :], 1.0)
            P.memset(KO[i][0:6, :], -1.0)
        S_.ensure(idx[("wqk", 1)])

        def fox_proj(pr):
            g, pp = pr // 2, pr % 2
            wq, wk = WQK[g]
            sl = 0
            for nt in range(4):
                cols = slice(nt * 512, (nt + 1) * 512)
                bq, bk = bank(0), bank(1)
                for kc in range(KC):
                    P.mm(bq, wq[:, kc, pp * 128:(pp + 1) * 128], XNT[:, kc, cols], start=(kc == 0), stop=(kc == KC - 1))
                for kc in range(KC):
                    P.mm(bk, wk[:, kc, pp * 128:(pp + 1) * 128], XNT[:, kc, cols], start=(kc == 0), stop=(kc == KC - 1))
                evac(QE[sl][0:64, cols], bq[0:64, :], 0, scale=FD ** -0.5)
                evac(QO[sl][64:128, cols], bq[64:128, :], 1, scale=FD ** -0.5)
                evac(KE[sl][0:64, cols], bk[0:64, :], 0)
                evac(KO[sl][64:128, cols], bk[64:128, :], 1)
            he, ho = 2 * pr, 2 * pr + 1
            for r in range(3):
                P.dma(QE[sl][64 + r: 65 + r, :], CS[r][he: he + 1, :], "aug%d" % sl)
                P.dma(KE[sl][67 + r: 68 + r, :], CS[r][he: he + 1, :], "aug%d" % sl)
                P.dma(QO[sl][r: r + 1, :], CS[r][ho: ho + 1, :], "aug%d" % sl)
                P.dma(KO[sl][3 + r: 4 + r, :], CS[r][ho: ho + 1, :], "aug%d" % sl)

        steps = []
        gidx_of = {}
        for h in range(FH):
            for qs in (3, 0, 2, 1):
                gidx_of[(h, qs)] = len(gidx_of)
                for kt in range(4 * qs + 4):
                    steps.append((h, qs, kt))

        def s_geom(i):
            h, qs, kt = steps[i]
            r = kt - 4 * qs
            cq0 = max(0, r) * 128
            return h, qs, kt, r, cq0, 512 - cq0

        def emit_s(i):
            h, qs, kt, r, cq0, n = s_geom(i)
            sl = 0
            if h % 2 == 0:
                qa, ka, rows = QE[sl], KE[sl], slice(0, 70)
            else:
                qa, ka, rows = QO[sl], KO[sl], slice(0, 128)
            sb_ = bank(2 + i % 2)
            P.mm(sb_[:, 0:n], ka[rows, kt * 128:(kt + 1) * 128], qa[rows, qs * 512 + cq0: qs * 512 + 512])

        def emit_rest(i):
            h, qs, kt, r, cq0, n = s_geom(i)
            hp = (h % 2) * 64
            op_ = 64 - hp
            last = 4 * qs + 3
            sb_ = bank(2 + i % 2)
            pt = PT[i % 4]
            nb = bank(4 + gidx_of[(h, qs)] % 3)
            P.act(pt[:, 0:n], sb_[:, 0:n], AF.Exp)
            if r >= 0:
                P.affine_select(pt[:, 0:128], pt[:, 0:128], [[1, 128]], ALU.is_ge, 0.0, 0, -1)
            vf = (VF2A if kt < 8 else VF2B)[:, kt % 8, h, :]
            P.mm(nb[:, cq0:512], vf, pt[:, 0:n], start=(kt == 0), stop=(kt == last))
            if kt == last:
                P.recip(RD[op_:op_ + 64, :], nb[op_:op_ + 64, :])
                P.dma(RD2[hp:hp + 64, :], RD[op_:op_ + 64, :], "rdmv")
                P.tt(AOT[hp:hp + 64, h // 2, qs * 512:(qs + 1) * 512], nb[hp:hp + 64, :], RD2[hp:hp + 64, :], ALU.mult)

        fox_proj(0)
        emit_s(0)
        for i in range(len(steps)):
            if i + 1 < len(steps):
                h1, qs1, kt1 = steps[i + 1]
                if h1 % 2 == 0 and steps[i][0] != h1:
                    fox_proj(h1 // 2)
                emit_s(i + 1)
            emit_rest(i)
        dump("AOT", AOT); dump("CS0", CS[0]); dump("CS1", CS[1]); dump("CS2", CS[2])
        S_.ensure(idx["wmv"])
        for t in range(NT):
            b = bank(t % 2)
            for kc in range(KC):
                P.mm(b, XNT[:, kc, t * 128:(t + 1) * 128], WMV[:, kc, :], start=(kc == 0), stop=(kc == KC - 1))
            evac(VM[:, t, :], b, t)
        P.dma(GW32[:], win_v[:, :, C_MI: C_MI + 8], "c0")
        k = 0
        for c in range(4):
            S_.ensure(idx[("wo_", c)])
            S_.prefetch_after(idx[("wo_", c)], idx[("wqkm", MH - 1, 1)])
            wb = WB[k % 2]
            k += 1
            for nt in range(4):
                b = bank(2 + nt % 2)
                for kc in range(KC):
                    P.mm(b, wb[:, kc, :], XNT[:, kc, nt * 512:(nt + 1) * 512], start=(kc == 0), stop=(kc == KC - 1))
                P.act(SO[:, c, nt * 512:(nt + 1) * 512], b, AF.Sigmoid)
        P.memset(HIST[:], 0.0)
        TG = [(T0, T1), (T0B, T1B)]
        PCs, CVs = [PCQ, PCK], [CVQ, CVK]
        its = [(h, nt) for h in range(MH) for nt in range(4)]
        kbase = k

        def prep_pe(i):
            h, nt = its[i]
            par = i % 2
            cols = slice(nt * 512, (nt + 1) * 512)
            if nt == 0:
                P.copy(WR[0][:], GW32[:, :, h: h + 1].to_broadcast([128, KC, 128]))
                P.copy(WR[1][:], GW32[:, :, 4 + h: 5 + h].to_broadcast([128, KC, 128]))
                for qk in range(2):
                    S_.ensure(idx[("wqkm", h, qk)])
            bf_, bi_ = bank(4 * par), bank(4 * par + 1)
            for kc in range(KC):
                P.mm(bf_, WR[1][:, kc, :], XNT[:, kc, cols], start=(kc == 0), stop=(kc == KC - 1))
            for kc in range(KC):
                P.mm(bi_, WR[0][:, kc, :], XNT[:, kc, cols], start=(kc == 0), stop=(kc == KC - 1))
            for qk in range(2):
                wb = WB[(kbase + 2 * h + qk) % 2]
                b = bank(4 * par + 2 + qk)
                for kc in range(KC):
                    P.mm(b, wb[:, kc, :], XNT[:, kc, cols], start=(kc == 0), stop=(kc == KC - 1))
                if nt == 3 and h + 1 < MH:
                    S_.ensure(idx[("wqkm", h + 1, qk)])

        def prep_gates(i):
            h, nt = its[i]
            par = i % 2
            t0, t1 = TG[par]
            bf_, bi_ = bank(4 * par), bank(4 * par + 1)
            P.act(t0[:], bf_, AF.Exp, bias=NBIAS[:, 12 + h: 13 + h], scale=-1.0)
            P.act(t0[:], t0[:], AF.Ln, bias=CONSTS[:, 1:2])
            P.scan(t1[:], MASKR[:], t0[:], 0.0, ALU.mult, ALU.add)
            P.act(EG[:, h, nt * 4:(nt + 1) * 4], t1[:, 127:512:128], AF.Exp, scale=-1.0)
            P.act(t0[:], t1[:], AF.Exp, bias=CONSTS[:, 0:1], scale=-1.0)
            P.tt(t1[:], t1[:], bi_, ALU.add)
            P.act(t1[:], t1[:], AF.Exp, bias=BIAS[:, 8 + h: 9 + h])

        def prep_conv(i):
            h, nt = its[i]
            par = i % 2
            cols = slice(nt * 512, (nt + 1) * 512)
            t0, t1 = TG[par]
            cw = lambda c, j: CONVW[:, c * 5 + j: c * 5 + j + 1]
            for qk in range(2):
                c = qk * 4 + h
                b = bank(4 * par + 2 + qk)
                pc, cv = PCs[qk], CVs[qk]
                P.copy(pc[:, 0:3], HIST[:, c, 0:3], eng="dve")
                P.copy(pc[:, 3:515], b, eng="act")
                P.act(cv[:], b, AF.Identity, bias=cw(c, 4), scale=cw(c, 3))
            for qk in range(2):
                c = qk * 4 + h
                pc, cv = PCs[qk], CVs[qk]
                for j in (2, 1, 0):
                    P.stt(cv[:], pc[:, j: j + 512], cw(c, j), cv[:], ALU.mult, ALU.add)
                P.copy(HIST[:, c, 0:3], pc[:, 512:515], eng="dve")
                P.act(cv[:], cv[:], AF.Silu)
            for qk in range(2):
                dst = (QTP if qk == 0 else KTP)[:, h, cols]
                P.tt(dst, CVs[qk][:], (t0 if qk == 0 else t1)[:], ALU.mult)

        prep_pe(0)
        prep_gates(0)
        for i in range(len(its)):
            if i + 1 < len(its):
                prep_pe(i + 1)
                prep_gates(i + 1)
            prep_conv(i)
        dump("QTP", QTP); dump("KTP", KTP); dump("VM", VM); dump("SOG", SO); dump("EG", EG)
        P.memset(C32[:], 0.0)
        P.memset(CB[:], 0.0)
        psb = ps[:].bitcast(BF16)
        STMs, KTOKs = [STM, STM2], [KTOK, KTOK2]
        TA, TB = [T0, TC], [T1, TD]
        dcb = ps[:, 4 * 512: 6 * 512].rearrange("p (h n) -> p h n", n=256)
        nb, db = bank(2), bank(3)

        def rec_pre(c):
            cols = slice(c * 128, (c + 1) * 128)
            stb = bank(0 if c % 2 == 0 else 6)
            kb = 1 if c % 2 == 0 else 7
            ktb_bf = psb[:, kb * 1024: kb * 1024 + 512].rearrange("p (h n) -> p h n", n=128)
            for h in range(MH):
                P.mm(stb[:, h * 128:(h + 1) * 128], KTP[:, h, cols], QTP[:, h, cols])
            for h in range(MH):
                P.transpose(ktb_bf[:, h, :], KTP[:, h, cols], IDENTB[:])
            P.tt(STMs[c % 2][:], stb.rearrange("p (h n) -> p h n", n=128),
                 MASKU[:].unsqueeze(1).to_broadcast([128, MH, 128]), ALU.mult)
            for h in range(MH):
                o_, i_, sc_ = KTOKs[c % 2][:, h, :], ktb_bf[:, h, :], EG[:, h, c: c + 1]
                P.add("act", (lambda e, o_=o_, i_=i_, sc_=sc_: e.mul(o_, i_, sc_)), reads=[i_, sc_], writes=[o_])

        rec_pre(0)
        for c in range(NT):
            cols = slice(c * 128, (c + 1) * 128)
            stm, ktok = STMs[c % 2], KTOKs[c % 2]
            ta, tb = TA[c % 2], TB[c % 2]
            if c + 1 < NT:
                rec_pre(c + 1)
            if c < NT - 1:
                for h in range(MH):
                    P.mm(dcb[:, h, 0:128], ktok[:, h, :], VM[:, c, h * 128:(h + 1) * 128])
                    P.mm(dcb[:, h, 128:256], ktok[:, h, :], ONESB[:, 0:128])
            for h in range(MH):
                P.mm(nb[:, h * 128:(h + 1) * 128], VM[:, c, h * 128:(h + 1) * 128], stm[:, h, :], start=True, stop=False)
                P.mm(nb[:, h * 128:(h + 1) * 128], CB[:, h, 0:128], QTP[:, h, cols], start=False, stop=True)
                P.mm(db[:, h * 128:(h + 1) * 128], ONESB[:, 0:128], stm[:, h, :], start=True, stop=False)
                P.mm(db[:, h * 128:(h + 1) * 128], CB[:, h, 128:256], QTP[:, h, cols], start=False, stop=True)
            if c < NT - 1:
                for h in range(MH):
                    P.stt(C32[:, h, :], C32[:, h, :], EG[:, h, c: c + 1], dcb[:, h, :], ALU.mult, ALU.add)
                P.copy(CB[:], C32[:], eng="act")
            P.act(ta[:], db, AF.Abs)
            P.ts(ta[:], ta[:], 1.0, None, ALU.max)
            P.act(ta[:], ta[:], AF.Ln)
            P.act(ta[:], ta[:], AF.Exp, scale=-1.0)
            P.tt(tb[:], nb, ta[:], ALU.mult)
            P.tt(SO[:, :, cols], tb[:].rearrange("p (h n) -> p h n", n=128), SO[:, :, cols], ALU.mult)
        dump("HM", SO)
        dump("R_QTP", QTP); dump("R_KTP", KTP); dump("R_VM", VM); dump("R_C32", C32); dump("R_CB", CB); dump("R_HM", SO)
        for c in range(KC):
            S_.ensure(idx[("wmg", c)])
            S_.prefetch_after(idx[("wmg", c)], idx[("wmg", KC - 1)])
            w = WMG[c % 2]
            for st in range(4):
                cols = slice(st * 512, (st + 1) * 512)
                b0 = 4 * ((c * 4 + st) % 2)
                ab, bb, gab, gbb = bank(b0), bank(b0 + 1), bank(b0 + 2), bank(b0 + 3)
                for kc in range(4):
                    P.mm(ab, w["uf"][:, kc, :], AOT[:, kc, cols], start=(kc == 0), stop=(kc == 3))
                for kc in range(4):
                    P.mm(bb, w["um"][:, kc, :], SO[:, kc, cols], start=(kc == 0), stop=(kc == 3))
                for kc in range(KC):
                    P.mm(gab, w["ga"][:, kc, :], XNT[:, kc, cols], start=(kc == 0), stop=(kc == KC - 1))
                for kc in range(KC):
                    P.mm(gbb, w["gb"][:, kc, :], XNT[:, kc, cols], start=(kc == 0), stop=(kc == KC - 1))
                P.act(SGA[:], gab, AF.Sigmoid)
                P.act(SGB[:], gbb, AF.Sigmoid)
                P.tt(M1[:], SGA[:], ab, ALU.mult)
                P.tt(M2[:], SGB[:], bb, ALU.mult)
                P.tt(MT[:, c, cols], M1[:], M2[:], ALU.add)
        dump("MT", MT)
        S_.ensure(idx["wo"])
        for t in range(NT):
            for dh in range(2):
                yb = bank((t * 2 + dh) % 2)
                for c in range(KC):
                    P.mm(yb, MT[:, c, t * 128:(t + 1) * 128], WO[:, c, dh * 512:(dh + 1) * 512],
                         start=(c == 0), stop=(c == KC - 1))
                xsl = X[:, t, dh * 512:(dh + 1) * 512]
                P.tt(xsl, yb, xsl, ALU.add)
        dump("L_AOT", AOT); dump("L_HM", SO); dump("L_MT", MT); dump("L_EG", EG); dump("L_XNT", XNT)

    x_v = x_d.rearrange("(s t p) d -> s p t d", p=128, t=NT)
    out_v = out_d.rearrange("(s t p) d -> s p t d", p=128, t=NT)

    sched = []
    for s in range(n_seq):
        if do_ffn1:
            sched.append(("ffn", s, 0, ffn_prepare(w1i_d, w1o_d)))
        if do_mix:
            sched.append(("mix", s, 1, mix_prepare()))
        if do_ffn2:
            sched.append(("ffn", s, 2, ffn_prepare(w2i_d, w2o_d)))
        sched.append(("fin", s, None, None))

    fused_final = do_ffn2
    prefetched = set()
    for (kind, s, gidx, idx) in sched:
        first_stage = (kind == "ffn" and gidx == 0) or (kind == "mix" and not do_ffn1) or \
                      (kind == "ffn" and gidx == 2 and not do_ffn1 and not do_mix) or \
                      (kind == "fin" and not (do_ffn1 or do_mix or do_ffn2))
        if first_stage:
            for t in range(NT):
                if (s, t) not in prefetched:
                    P.dma(X[:, t, :], x_v[s, :, t, :], "xinA" if t < 8 else "xinB")
        if kind == "ffn":
            if gidx == 2 and fused_final:
                nxt = s + 1 if s + 1 < n_seq else None
                ffn_phase(s, gidx, idx, final=True, nxt=nxt)
                if nxt is not None:
                    prefetched.update((nxt, t) for t in range(8))
            else:
                ffn_phase(s, gidx, idx)
        elif kind == "mix":
            mixer_phase(s, idx)
        elif kind == "fin" and not fused_final:
            P.dma(GF[:], gfin_d[0:1, :].partition_broadcast(128), "c0")
            rms_rstd(list(range(NT)), 16)
            for t in range(NT):
                ob = XS[t % 2]
                P.stt(ob[:], X[:, t, :], STAT[:, 16 + t: 17 + t], GF[:], ALU.mult, ALU.mult)
                P.dma(out_v[s, :, t, :], ob[:], "xout")
    P.emit()
    return nc


N_CORES = 8


def _prep_shared(inp):
    f = lambda a: np.ascontiguousarray(np.asarray(a, dtype=np.float32))
    g = np.stack([f(inp["ffn1_norm"])[0], f(inp["mix_norm"])[0], f(inp["ffn2_norm"])[0]], 0)
    gfm = np.ascontiguousarray(g.reshape(3, KC, 128).transpose(2, 0, 1).reshape(128, 3 * KC))
    cw = f(inp["mlstm_conv_w"])[0]
    cb = f(inp["mlstm_conv_b"])[0]
    cv = np.concatenate([cw, cb[None, :]], 0)
    conv_fm = np.ascontiguousarray(cv.reshape(5, KC, 128).transpose(2, 1, 0).reshape(128, KC * 5))
    biases = np.concatenate([f(inp["fox_f_bias"])[0], f(inp["mlstm_i_bias"])[0], f(inp["mlstm_f_bias"])[0]])[None, :]
    return {
        "ffn1_w_in": f(inp["ffn1_w_in"])[0], "ffn1_w_out": f(inp["ffn1_w_out"])[0],
        "ffn2_w_in": f(inp["ffn2_w_in"])[0], "ffn2_w_out": f(inp["ffn2_w_out"])[0],
        "w_in": f(inp["w_in"])[0], "w_up_fox": f(inp["w_up_fox"])[0], "w_up_mlstm": f(inp["w_up_mlstm"])[0],
        "w_out": f(inp["w_out"])[0], "gains_fm": gfm, "final_norm": f(inp["final_norm"]).reshape(1, D),
        "conv_fm": conv_fm, "biases": np.ascontiguousarray(biases),
        "fbias_col": np.ascontiguousarray(f(inp["fox_f_bias"])[0].reshape(8, 1)),
    }


_NC_CACHE = {}


def kernel(**inputs):
    x = np.asarray(inputs["x"], dtype=np.float32)
    B = x.shape[0]
    n_seq = B // N_CORES
    shared = _prep_shared(inputs)
    key = n_seq
    if key not in _NC_CACHE:
        _NC_CACHE[key] = build_program(n_seq)
    nc = _NC_CACHE[key]
    in_maps = []
    for c in range(N_CORES):
        m = dict(shared)
        m["x"] = np.ascontiguousarray(x[c * n_seq:(c + 1) * n_seq].reshape(n_seq * SEQ, D))
        in_maps.append(m)
    res = run_bass_kernel_spmd(nc, in_maps, core_ids=list(range(N_CORES)))
    out = np.concatenate([np.asarray(r["out"]).reshape(n_seq, SEQ, D) for r in res.results], 0)
    return out.astype(np.float32)
```

```python
import numpy as np
import concourse.bass as bass
import concourse.mybir as mybir
from concourse.bass_utils import run_bass_kernel_spmd

F32 = mybir.dt.float32
BF16 = mybir.dt.bfloat16
AF = mybir.ActivationFunctionType
ALU = mybir.AluOpType
AX = mybir.AxisListType

DT_SIZE = {F32: 4, BF16: 2}
PAGE = 512
SB0 = 16896
SB_SIZE = 229376 - SB0


class _Op:
    __slots__ = ("fn", "waits", "signal", "eng", "chan", "tag")

    def __init__(self, fn, eng, chan):
        self.fn = fn
        self.waits = {}
        self.signal = False
        self.eng = eng
        self.chan = chan


class Prog:
    COMPUTE = ("pe", "act", "dve", "pool")

    def __init__(self, nc):
        self.nc = nc
        self.ops = {e: [] for e in ("pe", "act", "dve", "pool", "sp")}
        self.base = {}
        self.pages = {}
        self.ident_ops = {}
        self.known = {}
        self.chan_consumed = {}
        self.sb_top = 0
        self.chan_list = []

    def sb(self, name, shape, dtype, addr):
        size = int(np.prod(shape[1:])) * DT_SIZE[dtype]
        assert addr % 32 == 0, (name, addr)
        assert addr + size <= SB_SIZE, (name, addr, size)
        addr = addr + SB0
        t = self.nc.alloc_sbuf_tensor_at(name, list(shape), dtype, offset=addr)
        self.base[t.name] = ("sb", addr)
        return t

    def reg_psum(self, t):
        self.base[t.name] = ("ps", 0)

    def reg_dram(self, ap, name=None):
        self.base[ap.tensor.name] = ("d:" + ap.tensor.name, 0)

    def region(self, ap):
        tn = ap.tensor.name
        space, base = self.base[tn]
        dims = ap.ap
        esz = DT_SIZE.get(ap.dtype, 4)
        off = int(ap.offset)
        if space.startswith("d:"):
            ext = 1
            for st, cnt in dims:
                ext += (cnt - 1) * abs(st)
            return (space, off * esz, (off + ext) * esz, 0, 1)
        pstep = dims[0][0]
        if pstep == 0:
            pstep = 1 << 40
        plo = off // pstep
        flo = off % pstep
        ext = 1
        for st, cnt in dims[1:]:
            ext += (cnt - 1) * abs(st)
        lo = base + flo * esz
        hi = base + (flo + ext) * esz
        phi = plo + dims[0][1]
        if space == "ps":
            lo = (lo // 2048) * 2048
            hi = ((hi + 2047) // 2048) * 2048
            plo = (plo // 32) * 32
            phi = ((phi + 31) // 32) * 32
        return (space, lo, hi, plo, phi)

    def _ident_idx(self, op_eng, chan):
        ident = chan if chan is not None else op_eng
        lst = self.ident_ops.setdefault(ident, [])
        return ident, lst

    def _add_dep(self, op, ident, idx):
        if ident.startswith("dma:"):
            idx = len(self.ident_ops[ident])
            if self.chan_consumed.get(ident, 0) < idx:
                self.chan_consumed[ident] = idx
        kn = self.known.setdefault(op.eng, {})
        if kn.get(ident, 0) >= idx:
            return
        if op.waits.get(ident, 0) < idx:
            op.waits[ident] = idx

    def add(self, eng, fn, reads=(), writes=(), chan=None):
        op = _Op(fn, eng, chan)
        op.tag = (writes[0].tensor.name if writes else "", len(self.ops[eng]))
        ident, lst = self._ident_idx(eng, chan)
        my_idx = len(lst) + 1
        racc = [self.region(a) for a in reads]
        wacc = [self.region(a) for a in writes]
        wacc += [a for a in racc if a[0] == "ps"]
        racc = [a for a in racc if a[0] != "ps"]
        same_ok_all = (ident == "pe")
        if chan is not None:
            k = self.chan_consumed.get(ident, 0)
            if k > 0:
                kn0 = self.known.setdefault(eng, {})
                if kn0.get(ident, 0) < k:
                    op.waits[ident] = k
        for kindnew, accs in (("R", racc), ("W", wacc)):
            for (space, lo, hi, plo, phi) in accs:
                for pg in range(lo // PAGE, (hi - 1) // PAGE + 1):
                    recs = self.pages.get((space, pg))
                    if not recs:
                        continue
                    for (rid, rkind), rec in recs.items():
                        if kindnew == "R" and rkind == "R":
                            continue
                        ridx, rlo, rhi, rplo, rphi = rec
                        if rlo >= hi or rhi <= lo or rplo >= phi or rphi <= plo:
                            continue
                        if rid == ident:
                            if same_ok_all:
                                continue
                            if ident.startswith("dma:"):
                                if rkind == "W" and kindnew == "W":
                                    continue
                            else:
                                if not (rkind == "W" and kindnew == "R"):
                                    continue
                        self._add_dep(op, rid, ridx)
        for kindnew, accs in (("R", racc), ("W", wacc)):
            for (space, lo, hi, plo, phi) in accs:
                for pg in range(lo // PAGE, (hi - 1) // PAGE + 1):
                    recs = self.pages.setdefault((space, pg), {})
                    pglo, pghi = pg * PAGE, (pg + 1) * PAGE
                    clo, chi = max(lo, pglo), min(hi, pghi)
                    if kindnew == "W":
                        for key in [k for k, r in recs.items()
                                    if max(r[1], pglo) >= clo and min(r[2], pghi) <= chi
                                    and r[3] >= plo and r[4] <= phi]:
                            del recs[key]
                    key = (ident, kindnew)
                    r = recs.get(key)
                    if r is None:
                        recs[key] = [my_idx, clo, chi, plo, phi]
                    else:
                        r[0] = my_idx
                        r[1] = min(r[1], clo); r[2] = max(r[2], chi)
                        r[3] = min(r[3], plo); r[4] = max(r[4], phi)
        kn = self.known.setdefault(eng, {})
        for wid, widx in op.waits.items():
            kn[wid] = max(kn.get(wid, 0), widx)
            tgt = self.ident_ops[wid][widx - 1]
            tgt.signal = True
        lst.append(op)
        self.ops[eng].append(op)
        return op

    def emit(self, final_waits=()):
        nc = self.nc
        idents = list(self.ident_ops.keys())
        from contextlib import ExitStack
        with ExitStack() as es:
            sems = {}
            for ident in idents:
                sems[ident] = es.enter_context(nc.semaphore("s_" + ident.replace(":", "_")))
            tick = {}
            for ident in idents:
                cnt = 0
                tl = []
                isdma = ident.startswith("dma:")
                for op in self.ident_ops[ident]:
                    if isdma:
                        cnt += 16
                    elif op.signal:
                        cnt += 1
                    tl.append(cnt)
                tick[ident] = tl
                assert cnt < 60000, (ident, cnt)
            block = es.enter_context(nc.Block())

            def run(engname, e):
                for op in self.ops[engname]:
                    for wid, widx in op.waits.items():
                        e.wait_ge(sems[wid], tick[wid][widx - 1])
                    ins = op.fn(e)
                    if op.chan is not None:
                        ins.then_inc(sems[op.chan], 16)
                    elif op.signal:
                        ins.then_inc(sems[op.eng], 1)

            @block.tensor
            def _(e):
                run("pe", e)

            @block.scalar
            def _(e):
                run("act", e)

            @block.vector
            def _(e):
                run("dve", e)

            @block.gpsimd
            def _(e):
                run("pool", e)

            @block.sync
            def _(e):
                run("sp", e)
                for ident in idents:
                    if tick[ident] and tick[ident][-1] > 0 and (ident.startswith("dma:")):
                        e.wait_ge(sems[ident], tick[ident][-1])

    def dma(self, out, in_, chan, q="sp"):
        ch = "dma:" + chan
        return self.add(q, lambda e: e.dma_start(out=out, in_=in_), reads=[in_], writes=[out], chan=ch)

    def mm(self, out, lhsT, rhs, start=True, stop=True, **kw):
        rd = [lhsT, rhs] + ([] if start else [out])
        return self.add("pe", lambda e: e.matmul(out, lhsT, rhs, start=start, stop=stop, **kw),
                        reads=rd, writes=[out])

    def transpose(self, out, in_, ident):
        return self.add("pe", lambda e: e.transpose(out, in_, ident), reads=[in_, ident], writes=[out])

    def act(self, out, in_, func, bias=None, scale=None, accum_out=None, eng="act"):
        rd = [in_]
        kw = {}
        if bias is not None:
            kw["bias"] = bias
            if not isinstance(bias, (int, float)):
                rd.append(bias)
        if scale is not None:
            kw["scale"] = scale
            if not isinstance(scale, (int, float)):
                rd.append(scale)
        wr = [out]
        if accum_out is not None:
            kw["accum_out"] = accum_out
            wr.append(accum_out)
        return self.add("act", lambda e: e.activation(out, in_, func, **kw), reads=rd, writes=wr)

    def tt(self, out, in0, in1, op, eng="dve"):
        return self.add(eng, lambda e: e.tensor_tensor(out, in0, in1, op), reads=[in0, in1], writes=[out])

    def ts(self, out, in0, s1, s2, op0, op1=None, eng="dve", accum_out=None):
        rd = [in0] + [s for s in (s1, s2) if s is not None and not isinstance(s, (int, float))]
        wr = [out] + ([accum_out] if accum_out is not None else [])
        if op1 is None:
            return self.add(eng, lambda e: e.tensor_single_scalar(out, in0, s1, op0), reads=rd, writes=wr)
        kw = {}
        if accum_out is not None:
            kw["accum_out"] = accum_out
        return self.add(eng, lambda e: e.tensor_scalar(out, in0, s1, s2, op0, op1, **kw), reads=rd, writes=wr)

    def stt(self, out, in0, scalar, in1, op0, op1, eng="dve"):
        rd = [in0, in1] + ([] if isinstance(scalar, (int, float)) else [scalar])
        return self.add(eng, lambda e: e.scalar_tensor_tensor(out, in0, scalar, in1, op0, op1),
                        reads=rd, writes=[out])

    def copy(self, out, in_, eng="dve"):
        if eng == "act":
            return self.add("act", lambda e: e.copy(out, in_), reads=[in_], writes=[out])
        return self.add(eng, lambda e: e.tensor_copy(out, in_), reads=[in_], writes=[out])

    def memset(self, out, val, eng="pool"):
        return self.add(eng, lambda e: e.memset(out, val), reads=[], writes=[out])

    def recip(self, out, in_):
        return self.add("dve", lambda e: e.reciprocal(out, in_), reads=[in_], writes=[out])

    def scan(self, out, d0, d1, init, op0, op1):
        rd = [d0, d1] + ([] if isinstance(init, (int, float)) else [init])
        return self.add("dve", lambda e: e.tensor_tensor_scan(out, d0, d1, init, op0, op1),
                        reads=rd, writes=[out])

    def affine_select(self, out, in_, pattern, cmp, fill, base, cm):
        return self.add("pool", lambda e: e.affine_select(out, in_, pattern, cmp, fill, base=base,
                                                          channel_multiplier=cm),
                        reads=[in_], writes=[out])


D = 1024
SEQ = 2048
NT = SEQ // 128
DFF = 2816
NJ = DFF // 128
KC = D // 128
FH, FD = 8, 64
MH, MD = 4, 128
INC = 5648
C_FQ, C_FK, C_FV, C_FF = 0, 512, 1024, 1536
C_MQK, C_MV, C_MI, C_MF, C_MO, C_G = 1544, 2568, 3080, 3084, 3088, 3600
EPS = 1e-6
KB = 1024


class Stream:
    def __init__(self):
        self.items = []
        self.issued = 0

    def push(self, thunk, major=True):
        self.items.append((thunk, major))
        return len(self.items) - 1

    def ensure(self, k):
        k = min(k, len(self.items) - 1)
        while self.issued <= k:
            self.items[self.issued][0]()
            self.issued += 1

    def prefetch_after(self, k, limit):
        j = k + 1
        while j < len(self.items) and not self.items[j][1]:
            j += 1
        self.ensure(min(j, limit))


def build_program(n_seq, do_ffn1=True, do_mix=True, do_ffn2=True, dbg=None):
    nc = bass.Bass("TRN2", target_bir_lowering=False)
    P = Prog(nc)

    def din(name, shape):
        a = nc.dram_tensor(name, list(shape), F32, kind="ExternalInput").ap()
        P.reg_dram(a)
        return a

    x_d = din("x", [n_seq * SEQ, D])
    w1i_d = din("ffn1_w_in", [D, 2 * DFF])
    w1o_d = din("ffn1_w_out", [DFF, D])
    w2i_d = din("ffn2_w_in", [D, 2 * DFF])
    w2o_d = din("ffn2_w_out", [DFF, D])
    win_d = din("w_in", [D, INC])
    wuf_d = din("w_up_fox", [512, D])
    wum_d = din("w_up_mlstm", [512, D])
    wo_d = din("w_out", [D, D])
    gfm_d = din("gains_fm", [128, 3 * KC])
    gfin_d = din("final_norm", [1, D])
    convw_d = din("conv_fm", [128, KC * 5])
    bias_d = din("biases", [1, 16])
    fbc_d = din("fbias_col", [8, 1])
    out_d = nc.dram_tensor("out", [n_seq * SEQ, D], F32, kind="ExternalOutput").ap()
    P.reg_dram(out_d)

    ps = nc.alloc_psum_tensor("ps", [128, 4096], F32)
    P.reg_psum(ps)

    def bank(b, n=512, off=0):
        return ps[:, b * 512 + off: b * 512 + off + n]

    X = P.sb("X", [128, NT, D], F32, 0)
    MISC = 184 * KB
    XS = [P.sb("XS%d" % i, [128, D], F32, MISC + i * 4 * KB) for i in range(2)]
    SG = [P.sb("SG%d" % i, [128, 512], F32, MISC + 8 * KB + i * 2 * KB) for i in range(2)]
    GF = P.sb("GF", [128, D], F32, MISC + 12 * KB)
    JUNK = P.sb("JUNK", [128, D], BF16, MISC + 16 * KB)
    IDENT = P.sb("IDENT", [128, 128], F32, MISC + 18 * KB)
    IDENTB = P.sb("IDENTB", [128, 128], BF16, MISC + 18 * KB + 512)
    SM = MISC + 19 * KB
    GFM = P.sb("GFM", [128, 3 * KC], F32, SM)
    STAT = P.sb("STAT", [128, 64], F32, SM + 128)
    CONVW = P.sb("CONVW", [128, KC * 5], F32, SM + 384)
    BIAS = P.sb("BIAS", [128, 16], F32, SM + 544)
    NBIAS = P.sb("NBIAS", [128, 16], F32, SM + 608)
    ONESB = P.sb("ONESB", [128, 128], BF16, SM + 704)
    XNTH = P.sb("XNTH", [128, KC, 1024], BF16, 64 * KB)
    AT = P.sb("AT", [128, NJ, 1024], BF16, 80 * KB)
    WOUT = P.sb("WOUT", [128, NJ, D], BF16, 124 * KB)
    WBLK = [(P.sb("WG%d" % i, [128, KC, 256], BF16, 168 * KB + i * 8 * KB),
             P.sb("WU%d" % i, [128, KC, 256], BF16, 172 * KB + i * 8 * KB)) for i in range(2)]

    P.dma(GFM[:], gfm_d[:, :], "c0")
    P.dma(CONVW[:], convw_d[:, :], "c0")
    P.dma(BIAS[:], bias_d[0:1, :].partition_broadcast(128), "c0")
    P.memset(IDENT[:], 1.0)
    P.affine_select(IDENT[:], IDENT[:], [[1, 128]], ALU.is_equal, 0.0, 0, -1)
    P.copy(IDENTB[:], IDENT[:], eng="pool")
    P.memset(ONESB[:], 1.0)
    P.ts(NBIAS[:], BIAS[:], -1.0, None, ALU.mult)

    def rms_rstd(tiles, col0):
        n = len(tiles)
        for i, t in enumerate(tiles):
            P.act(JUNK[:], X[:, t, :], AF.Square, accum_out=STAT[:, col0 + i: col0 + i + 1])
        sl = STAT[:, col0: col0 + n]
        P.ts(sl, sl, 1.0 / D, EPS, ALU.mult, ALU.add)
        P.act(sl, sl, AF.Sqrt)
        P.recip(sl, sl)

    def norm_transpose(t, rcol, gidx, dst, dcol, nbuf):
        xs = XS[nbuf % 2]
        rs = STAT[:, rcol: rcol + 1]
        P.add("act", lambda e: e.mul(xs[:], X[:, t, :], rs), reads=[X[:, t, :], rs], writes=[xs[:]])
        pb = 0 if nbuf % 2 == 0 else 2 * 512
        for c in range(KC):
            P.transpose(ps[:, pb + c * 128: pb + (c + 1) * 128], xs[:, c * 128:(c + 1) * 128], IDENT[:])
        g = GFM[:, gidx * KC:(gidx + 1) * KC].unsqueeze(2).to_broadcast([128, KC, 128])
        P.tt(dst[:, :, dcol: dcol + 128], ps[:, pb: pb + 1024].rearrange("p (c l) -> p c l", l=128), g, ALU.mult)

    pool_stream = Stream()

    def ffn_prepare(w_in_d, w_out_d):
        w_in_v = w_in_d.rearrange("(kc p) n -> p kc n", p=128)
        w_out_v = w_out_d.rearrange("(j p) n -> p j n", p=128)
        idx = {}

        def mk_blk(jb, slot_i):
            def th():
                wg, wu = WBLK[slot_i]
                P.dma(wg[:], w_in_v[:, :, jb * 256:(jb + 1) * 256], "wblk%d" % slot_i, q="pool")
                P.dma(wu[:], w_in_v[:, :, DFF + jb * 256: DFF + (jb + 1) * 256], "wblk%d" % slot_i, q="pool")
            return th

        def mk_wout(j0, j1):
            def th():
                P.dma(WOUT[:, j0:j1, :], w_out_v[:, j0:j1, :], "wout", q="pool")
            return th

        k = 0
        for hf in range(2):
            for jb in range(NJ // 2):
                idx[("blk", hf, jb)] = pool_stream.push(mk_blk(jb, k % 2))
                k += 1
                if hf == 0 and jb in (2, 4, 6, 8):
                    q = (jb // 2 - 1)
                    j0, j1 = [(0, 6), (6, 12), (12, 17), (17, 22)][q]
                    idx[("wout", q)] = pool_stream.push(mk_wout(j0, j1), major=False)
        return idx

    def final_norm_tile(s, t):
        sl = STAT[:, 16 + t: 17 + t]
        P.act(JUNK[:], X[:, t, :], AF.Square, accum_out=sl)
        P.ts(sl, sl, 1.0 / D, EPS, ALU.mult, ALU.add)
        P.act(sl, sl, AF.Sqrt)
        P.recip(sl, sl)
        ob = XS[t % 2]
        P.stt(ob[:], X[:, t, :], sl, GF[:], ALU.mult, ALU.mult)
        P.dma(out_v[s, :, t, :], ob[:], "xout")

    def ffn_phase(s, gidx, idx, final=False, nxt=None):
        slot = 0
        if final:
            P.dma(GF[:], gfin_d[0:1, :].partition_broadcast(128), "c0")
        rms_rstd(list(range(8)), 0)
        for i in range(8):
            norm_transpose(i, i, gidx, XNTH, i * 128, i)
        for hf in range(2):
            for jb in range(NJ // 2):
                cur = idx[("blk", hf, jb)]
                pool_stream.ensure(cur)
                pool_stream.prefetch_after(cur, idx[("blk", 1, NJ // 2 - 1)])
                wg, wu = WBLK[slot % 2]
                slot += 1
                for jj in range(2):
                    j = 2 * jb + jj
                    for nt in range(2):
                        pb = 2 + 2 * ((j * 2 + nt) % 2)
                        gb, ub = bank(pb), bank(pb + 1)
                        for kc in range(KC):
                            P.mm(gb, wg[:, kc, jj * 128:(jj + 1) * 128], XNTH[:, kc, nt * 512:(nt + 1) * 512],
                                 start=(kc == 0), stop=(kc == KC - 1))
                        for kc in range(KC):
                            P.mm(ub, wu[:, kc, jj * 128:(jj + 1) * 128], XNTH[:, kc, nt * 512:(nt + 1) * 512],
                                 start=(kc == 0), stop=(kc == KC - 1))
                        sg = SG[(j * 2 + nt) % 2]
                        P.act(sg[:], gb, AF.Silu)
                        P.tt(AT[:, j, nt * 512:(nt + 1) * 512], sg[:], ub, ALU.mult)
                if final and hf == 1 and nxt is not None and jb == 2:
                    for t in range(8):
                        P.dma(X[:, t, :], x_v[nxt, :, t, :], "xinA")
            pool_stream.ensure(idx[("wout", 3)])
            if hf == 0:
                rms_rstd(list(range(8, 16)), 8)
            for tt_ in range(8):
                t = hf * 8 + tt_
                for dh in range(2):
                    yb = bank(6 + (tt_ * 2 + dh) % 2)
                    for j in range(NJ):
                        P.mm(yb, AT[:, j, tt_ * 128:(tt_ + 1) * 128], WOUT[:, j, dh * 512:(dh + 1) * 512],
                             start=(j == 0), stop=(j == NJ - 1))
                    xsl = X[:, t, dh * 512:(dh + 1) * 512]
                    P.stt(xsl, yb, 0.5, xsl, ALU.mult, ALU.add)
                if hf == 0:
                    norm_transpose(8 + tt_, 8 + tt_, gidx, XNTH, tt_ * 128, tt_)
                if final:
                    final_norm_tile(s, t)

    import math
    XNT = P.sb("XNT", [128, KC, SEQ], BF16, 64 * KB)
    VF = P.sb("VF", [128, NT, 512], BF16, 96 * KB)
    AOT = P.sb("AOT", [128, 4, SEQ], BF16, 112 * KB)
    QE = [P.sb("QE%d" % i, [128, SEQ], BF16, (128 * KB, 184 * KB)[i]) for i in range(2)]
    KE = [P.sb("KE%d" % i, [128, SEQ], BF16, (132 * KB, 188 * KB)[i]) for i in range(2)]
    QO = [P.sb("QO%d" % i, [128, SEQ], BF16, (136 * KB, 192 * KB)[i]) for i in range(2)]
    KO = [P.sb("KO%d" % i, [128, SEQ], BF16, (140 * KB, 158 * KB)[i]) for i in range(2)]
    WV = P.sb("WV", [128, KC, 512], BF16, 128 * KB)
    PT = [P.sb("PT%d" % i, [128, 512], BF16, 144 * KB + i * KB) for i in range(4)]
    CF = [P.sb("CF%d" % i, [8, SEQ], F32, 148 * KB + i * 8 * KB) for i in range(2)]
    CS = [P.sb("CS%d" % i, [8, SEQ], BF16, 164 * KB + i * 4 * KB) for i in range(3)]
    WQK = [(P.sb("WQ4_0", [128, KC, 256], BF16, 176 * KB), P.sb("WK4_0", [128, KC, 256], BF16, 180 * KB)),
           (P.sb("WQ4_1", [128, KC, 256], BF16, 148 * KB), P.sb("WK4_1", [128, KC, 256], BF16, 152 * KB))]
    RD = P.sb("RD", [128, 512], F32, 156 * KB)
    RD2 = P.sb("RD2", [128, 512], F32, 158 * KB)
    VF2A = P.sb("VF2A", [128, 8, FH, 128], BF16, 96 * KB)
    VF2B = P.sb("VF2B", [128, 8, FH, 128], BF16, 184 * KB)
    QTP = P.sb("QTP", [128, MH, SEQ], BF16, 96 * KB)
    KTP = P.sb("KTP", [128, MH, SEQ], BF16, 128 * KB)
    VM = P.sb("VM", [128, NT, 512], BF16, 144 * KB)
    SO = P.sb("SO", [128, MH, SEQ], BF16, 160 * KB)
    WB = [P.sb("WB%d" % i, [128, KC, 128], BF16, 176 * KB + i * 2 * KB) for i in range(2)]
    WR = [P.sb("WR%d" % i, [128, KC, 128], BF16, 180 * KB + i * 2 * KB) for i in range(2)]
    WMV = P.sb("WMV", [128, KC, 512], BF16, 176 * KB)
    T0 = P.sb("T0", [128, 512], F32, 184 * KB)
    T1 = P.sb("T1", [128, 512], F32, 186 * KB)
    T0B = P.sb("T0B", [128, 512], F32, 188 * KB)
    T1B = P.sb("T1B", [128, 512], F32, 190 * KB)
    PCQ = P.sb("PCQ", [128, 520], F32, 192 * KB)
    CVQ = P.sb("CVQ", [128, 512], F32, 192 * KB + 2560)
    PCK = P.sb("PCK", [128, 520], F32, 192 * KB + 4608)
    CVK = P.sb("CVK", [128, 512], F32, 192 * KB + 7168)
    C32 = P.sb("C32", [128, MH, 256], F32, 176 * KB)
    CB = P.sb("CB", [128, MH, 256], BF16, 180 * KB)
    KTOK = P.sb("KTOK", [128, MH, 128], BF16, 182 * KB)
    STM = P.sb("STM", [128, MH, 128], BF16, 183 * KB)
    KTOK2 = P.sb("KTOK2", [128, MH, 128], BF16, 188 * KB)
    STM2 = P.sb("STM2", [128, MH, 128], BF16, 189 * KB)
    TC = P.sb("TC", [128, 512], F32, 190 * KB)
    TD = P.sb("TD", [128, 512], F32, 192 * KB)
    WO = P.sb("WO", [128, KC, D], BF16, 96 * KB)
    MT = P.sb("MT", [128, KC, SEQ], BF16, 128 * KB)
    WMG = [dict(uf=P.sb("WUF%d" % i, [128, 4, 128], BF16, 176 * KB + i * 6 * KB),
                um=P.sb("WUM%d" % i, [128, 4, 128], BF16, 177 * KB + i * 6 * KB),
                ga=P.sb("WGA%d" % i, [128, KC, 128], BF16, 178 * KB + i * 6 * KB),
                gb=P.sb("WGB%d" % i, [128, KC, 128], BF16, 180 * KB + i * 6 * KB)) for i in range(2)]
    SGA = P.sb("SGA", [128, 512], F32, 188 * KB)
    SGB = P.sb("SGB", [128, 512], F32, 190 * KB)
    M1 = P.sb("M1", [128, 512], F32, 192 * KB)
    M2 = P.sb("M2", [128, 512], F32, 194 * KB)
    C2 = 204 * KB
    MASKR = P.sb("MASKR", [128, 512], F32, C2)
    MASKNEG = P.sb("MASKNEG", [128, 128], F32, C2 + 2048)
    MASKU = P.sb("MASKU", [128, 128], BF16, C2 + 2560)
    GW32 = P.sb("GW32", [128, KC, 8], F32, C2 + 2816)
    EG = P.sb("EG", [128, MH, NT], F32, C2 + 3072)
    HIST = P.sb("HIST", [128, KC, 4], F32, C2 + 3328)
    CONSTS = P.sb("CONSTS", [128, 8], F32, C2 + 3456)
    FBC = P.sb("FBC", [8, 2], F32, MISC + 18 * KB + 896)
    WFG = P.sb("WFG2", [128, KC, 8], BF16, MISC + 18 * KB + 768)

    P.memset(MASKR[:], 1.0)
    P.memset(MASKR[:].rearrange("p (c l) -> p c l", l=128)[:, :, 0:1], 0.0)
    P.memset(MASKNEG[:], 0.0)
    P.affine_select(MASKNEG[:], MASKNEG[:], [[1, 128]], ALU.is_ge, -30000.0, 0, -1)
    P.memset(MASKU[:], 1.0)
    P.affine_select(MASKU[:], MASKU[:], [[1, 128]], ALU.is_ge, 0.0, 0, -1)
    P.memset(CONSTS[:, 0:1], math.log(MD ** -0.5))
    P.memset(CONSTS[:, 1:2], 1.0)
    P.dma(FBC[:, 0:1], fbc_d[:, :], "c0")
    P.ts(FBC[:, 1:2], FBC[:, 0:1], -1.0, None, ALU.mult)

    win_v = win_d.rearrange("(kc p) n -> p kc n", p=128)

    def wload(dst, col0, ncol, chan):
        P.dma(dst, win_v[:, :, col0: col0 + ncol], chan, q="pool")

    def mix_prepare():
        idx = {}
        S_ = pool_stream
        idx["wv"] = S_.push(lambda: (wload(WV[:], C_FV, 512, "mwa"), wload(WFG[:], C_FF, 8, "mwa")))
        for g in range(2):
            idx[("wqk", g)] = S_.push((lambda g=g: (wload(WQK[g][0][:], C_FQ + g * 256, 256, "wqk%d" % g),
                                                    wload(WQK[g][1][:], C_FK + g * 256, 256, "wqk%d" % g))))
        idx["wmv"] = S_.push(lambda: wload(WMV[:], C_MV, 512, "mwa"))
        k = 0
        for c in range(4):
            idx[("wo_", c)] = S_.push((lambda c=c, k=k: wload(WB[k % 2][:], C_MO + c * 128, 128, "wb%d" % (k % 2))))
            k += 1
        for h in range(MH):
            for qk in range(2):
                idx[("wqkm", h, qk)] = S_.push((lambda h=h, qk=qk, k=k: wload(WB[k % 2][:], C_MQK + (qk * 4 + h) * 128,
                                                                             128, "wb%d" % (k % 2))))
                k += 1
        wuf_v = wuf_d.rearrange("(kc p) n -> p kc n", p=128)
        wum_v = wum_d.rearrange("(kc p) n -> p kc n", p=128)
        wo_v = wo_d.rearrange("(kc p) n -> p kc n", p=128)
        for c in range(KC):
            def th(c=c):
                w = WMG[c % 2]
                ch = "wmg%d" % (c % 2)
                P.dma(w["uf"][:], wuf_v[:, :, c * 128:(c + 1) * 128], ch, q="pool")
                P.dma(w["um"][:], wum_v[:, :, c * 128:(c + 1) * 128], ch, q="pool")
                wload(w["ga"][:], C_G + c * 128, 128, ch)
                wload(w["gb"][:], C_G + D + c * 128, 128, ch)
            idx[("wmg", c)] = S_.push(th)
            if c == 1:
                idx["wo"] = S_.push(lambda: P.dma(WO[:], wo_v[:, :, :], "mwa", q="pool"), major=False)
        return idx

    def evac(dst, src, k, scale=None):
        if scale is None:
            P.copy(dst, src, eng=("act" if k % 2 == 0 else "dve"))
        elif k % 2 == 0:
            P.add("act", lambda e: e.mul(dst, src, scale), reads=[src], writes=[dst])
        else:
            P.ts(dst, src, scale, None, ALU.mult)

    dbg_outs = {}

    def dump(name, t):
        if not dbg or name not in dbg:
            return
        shape = list(t.shape)
        o = nc.dram_tensor("dbg_" + name, shape, t.dtype, kind="ExternalOutput").ap()
        P.reg_dram(o)
        P.dma(o, t[:], "dbg")

    def mixer_phase(s, idx):
        S_ = pool_stream
        S_.ensure(idx["wv"])
        rms_rstd(list(range(NT)), 32)
        for t in range(NT):
            norm_transpose(t, 32 + t, 1, XNT, t * 128, t)
        S_.ensure(idx[("wqk", 0)])
        for nt in range(4):
            b = bank(2 + nt % 2)
            for kc in range(KC):
                P.mm(b[0:8, :], WFG[:, kc, 0:8], XNT[:, kc, nt * 512:(nt + 1) * 512], start=(kc == 0), stop=(kc == KC - 1))
            P.act(CF[0][:, nt * 512:(nt + 1) * 512], b[0:8, :], AF.Exp, bias=FBC[:, 1:2], scale=-1.0)
        P.act(CF[0][:], CF[0][:], AF.Ln, bias=CONSTS[0:8, 1:2])
        P.scan(CF[1][:], CONSTS[0:8, 1:2].to_broadcast([8, SEQ]), CF[0][:], 0.0, ALU.mult, ALU.add)
        P.copy(CS[0][:], CF[1][:])
        P.tt(CF[0][:], CF[1][:], CS[0][:], ALU.subtract)
        P.copy(CS[1][:], CF[0][:])
        P.tt(CF[1][:], CF[0][:], CS[1][:], ALU.subtract)
        P.copy(CS[2][:], CF[1][:])
        P.memset(VF2A[:], 1.0)
        P.memset(VF2B[:], 1.0)
        for t in range(NT):
            b = bank(t % 2)
            for kc in range(KC):
                P.mm(b, XNT[:, kc, t * 128:(t + 1) * 128], WV[:, kc, :], start=(kc == 0), stop=(kc == KC - 1))
            vf = (VF2A if t < 8 else VF2B)[:, t % 8]
            bv = b.rearrange("p (h d) -> p h d", d=FD)
            evac(vf[:, 0::2, 0:64], bv[:, 0::2, :], 0)
            evac(vf[:, 1::2, 64:128], bv[:, 1::2, :], 1)
        for i in range(1):
            P.memset(QE[i][64:70, :], 1.0)
            P.memset(KE[i][64:70, :], -1.0)
            P.memset(QO[i][0:64, :], 0.0)
            P.memset(KO[i][0:64, :], 0.0)
            P.memset(QO[i][0:6, :], 1.0)
            P.memset(KO[i][0:6, :], -1.0)
        S_.ensure(idx[("wqk", 1)])

        def fox_proj(pr):
            g, pp = pr // 2, pr % 2
            wq, wk = WQK[g]
            sl = 0
            for nt in range(4):
                cols = slice(nt * 512, (nt + 1) * 512)
                bq, bk = bank(0), bank(1)
                for kc in range(KC):
                    P.mm(bq, wq[:, kc, pp * 128:(pp + 1) * 128], XNT[:, kc, cols], start=(kc == 0), stop=(kc == KC - 1))
                for kc in range(KC):
                    P.mm(bk, wk[:, kc, pp * 128:(pp + 1) * 128], XNT[:, kc, cols], start=(kc == 0), stop=(kc == KC - 1))
                evac(QE[sl][0:64, cols], bq[0:64, :], 0, scale=FD ** -0.5)
                evac(QO[sl][64:128, cols], bq[64:128, :], 1, scale=FD ** -0.5)
                evac(KE[sl][0:64, cols], bk[0:64, :], 0)
                evac(KO[sl][64:128, cols], bk[64:128, :], 1)
            he, ho = 2 * pr, 2 * pr + 1
            for r in range(3):
                P.dma(QE[sl][64 + r: 65 + r, :], CS[r][he: he + 1, :], "aug%d" % sl)
                P.dma(KE[sl][67 + r: 68 + r, :], CS[r][he: he + 1, :], "aug%d" % sl)
                P.dma(QO[sl][r: r + 1, :], CS[r][ho: ho + 1, :], "aug%d" % sl)
                P.dma(KO[sl][3 + r: 4 + r, :], CS[r][ho: ho + 1, :], "aug%d" % sl)

        steps = []
        gidx_of = {}
        for h in range(FH):
            for qs in (3, 0, 2, 1):
                gidx_of[(h, qs)] = len(gidx_of)
                for kt in range(4 * qs + 4):
                    steps.append((h, qs, kt))

        def s_geom(i):
            h, qs, kt = steps[i]
            r = kt - 4 * qs
            cq0 = max(0, r) * 128
            return h, qs, kt, r, cq0, 512 - cq0

        def emit_s(i):
            h, qs, kt, r, cq0, n = s_geom(i)
            sl = 0
            if h % 2 == 0:
                qa, ka, rows = QE[sl], KE[sl], slice(0, 70)
            else:
                qa, ka, rows = QO[sl], KO[sl], slice(0, 128)
            sb_ = bank((2, 3, 7)[i % 3])
            P.mm(sb_[:, 0:n], ka[rows, kt * 128:(kt + 1) * 128], qa[rows, qs * 512 + cq0: qs * 512 + 512])

        def emit_rest(i):
            h, qs, kt, r, cq0, n = s_geom(i)
            hp = (h % 2) * 64
            op_ = 64 - hp
            last = 4 * qs + 3
            sb_ = bank((2, 3, 7)[i % 3])
            pt = PT[i % 4]
            nb = bank(4 + gidx_of[(h, qs)] % 3)
            P.act(pt[:, 0:n], sb_[:, 0:n], AF.Exp)
            if r >= 0:
                P.affine_select(pt[:, 0:128], pt[:, 0:128], [[1, 128]], ALU.is_ge, 0.0, 0, -1)
            vf = (VF2A if kt < 8 else VF2B)[:, kt % 8, h, :]
            P.mm(nb[:, cq0:512], vf, pt[:, 0:n], start=(kt == 0), stop=(kt == last))
            if kt == last:
                P.recip(RD[op_:op_ + 64, :], nb[op_:op_ + 64, :])
                P.dma(RD2[hp:hp + 64, :], RD[op_:op_ + 64, :], "rdmv")
                P.tt(AOT[hp:hp + 64, h // 2, qs * 512:(qs + 1) * 512], nb[hp:hp + 64, :], RD2[hp:hp + 64, :], ALU.mult)

        fox_proj(0)
        emit_s(0)
        emit_s(1)
        for i in range(len(steps)):
            if i + 2 < len(steps):
                h2 = steps[i + 2][0]
                if h2 % 2 == 0 and steps[i + 1][0] != h2:
                    fox_proj(h2 // 2)
                emit_s(i + 2)
            emit_rest(i)
        dump("AOT", AOT); dump("CS0", CS[0]); dump("CS1", CS[1]); dump("CS2", CS[2])
        S_.ensure(idx["wmv"])
        for t in range(NT):
            b = bank(t % 2)
            for kc in range(KC):
                P.mm(b, XNT[:, kc, t * 128:(t + 1) * 128], WMV[:, kc, :], start=(kc == 0), stop=(kc == KC - 1))
            evac(VM[:, t, :], b, t)
        P.dma(GW32[:], win_v[:, :, C_MI: C_MI + 8], "c0")
        k = 0
        for c in range(4):
            S_.ensure(idx[("wo_", c)])
            S_.prefetch_after(idx[("wo_", c)], idx[("wqkm", MH - 1, 1)])
            wb = WB[k % 2]
            k += 1
            for nt in range(4):
                b = bank(2 + nt % 2)
                for kc in range(KC):
                    P.mm(b, wb[:, kc, :], XNT[:, kc, nt * 512:(nt + 1) * 512], start=(kc == 0), stop=(kc == KC - 1))
                P.act(SO[:, c, nt * 512:(nt + 1) * 512], b, AF.Sigmoid)
        P.memset(HIST[:], 0.0)
        TG = [(T0, T1), (T0B, T1B)]
        PCs, CVs = [PCQ, PCK], [CVQ, CVK]
        its = [(h, nt) for h in range(MH) for nt in range(4)]
        kbase = k

        def prep_pe(i):
            h, nt = its[i]
            par = i % 2
            cols = slice(nt * 512, (nt + 1) * 512)
            if nt == 0:
                P.copy(WR[0][:], GW32[:, :, h: h + 1].to_broadcast([128, KC, 128]))
                P.copy(WR[1][:], GW32[:, :, 4 + h: 5 + h].to_broadcast([128, KC, 128]))
                for qk in range(2):
                    S_.ensure(idx[("wqkm", h, qk)])
            bf_, bi_ = bank(4 * par), bank(4 * par + 1)
            for kc in range(KC):
                P.mm(bf_, WR[1][:, kc, :], XNT[:, kc, cols], start=(kc == 0), stop=(kc == KC - 1))
            for kc in range(KC):
                P.mm(bi_, WR[0][:, kc, :], XNT[:, kc, cols], start=(kc == 0), stop=(kc == KC - 1))
            for qk in range(2):
                wb = WB[(kbase + 2 * h + qk) % 2]
                b = bank(4 * par + 2 + qk)
                for kc in range(KC):
                    P.mm(b, wb[:, kc, :], XNT[:, kc, cols], start=(kc == 0), stop=(kc == KC - 1))
                if nt == 3 and h + 1 < MH:
                    S_.ensure(idx[("wqkm", h + 1, qk)])

        def prep_gates(i):
            h, nt = its[i]
            par = i % 2
            t0, t1 = TG[par]
            bf_, bi_ = bank(4 * par), bank(4 * par + 1)
            P.act(t0[:], bf_, AF.Exp, bias=NBIAS[:, 12 + h: 13 + h], scale=-1.0)
            P.act(t0[:], t0[:], AF.Ln, bias=CONSTS[:, 1:2])
            P.scan(t1[:], MASKR[:], t0[:], 0.0, ALU.mult, ALU.add)
            P.act(EG[:, h, nt * 4:(nt + 1) * 4], t1[:, 127:512:128], AF.Exp, scale=-1.0)
            P.act(t0[:], t1[:], AF.Exp, bias=CONSTS[:, 0:1], scale=-1.0)
            P.tt(t1[:], t1[:], bi_, ALU.add)
            P.act(t1[:], t1[:], AF.Exp, bias=BIAS[:, 8 + h: 9 + h])

        def prep_conv(i):
            h, nt = its[i]
            par = i % 2
            cols = slice(nt * 512, (nt + 1) * 512)
            t0, t1 = TG[par]
            cw = lambda c, j: CONVW[:, c * 5 + j: c * 5 + j + 1]
            for qk in range(2):
                c = qk * 4 + h
                b = bank(4 * par + 2 + qk)
                pc, cv = PCs[qk], CVs[qk]
                P.copy(pc[:, 0:3], HIST[:, c, 0:3], eng="dve")
                P.copy(pc[:, 3:515], b, eng="act")
                P.act(cv[:], b, AF.Identity, bias=cw(c, 4), scale=cw(c, 3))
            for qk in range(2):
                c = qk * 4 + h
                pc, cv = PCs[qk], CVs[qk]
                for j in (2, 1, 0):
                    P.stt(cv[:], pc[:, j: j + 512], cw(c, j), cv[:], ALU.mult, ALU.add)
                P.copy(HIST[:, c, 0:3], pc[:, 512:515], eng="dve")
                P.act(cv[:], cv[:], AF.Silu)
            for qk in range(2):
                dst = (QTP if qk == 0 else KTP)[:, h, cols]
                P.tt(dst, CVs[qk][:], (t0 if qk == 0 else t1)[:], ALU.mult)

        prep_pe(0)
        prep_gates(0)
        for i in range(len(its)):
            if i + 1 < len(its):
                prep_pe(i + 1)
                prep_gates(i + 1)
            prep_conv(i)
        dump("QTP", QTP); dump("KTP", KTP); dump("VM", VM); dump("SOG", SO); dump("EG", EG)
        P.memset(C32[:], 0.0)
        P.memset(CB[:], 0.0)
        psb = ps[:].bitcast(BF16)
        STMs, KTOKs = [STM, STM2], [KTOK, KTOK2]
        TA, TB = [T0, TC], [T1, TD]
        dcb = ps[:, 4 * 512: 6 * 512].rearrange("p (h n) -> p h n", n=256)
        nb, db = bank(2), bank(3)

        def rec_pre(c):
            cols = slice(c * 128, (c + 1) * 128)
            stb = bank(0 if c % 2 == 0 else 6)
            kb = 1 if c % 2 == 0 else 7
            ktb_bf = psb[:, kb * 1024: kb * 1024 + 512].rearrange("p (h n) -> p h n", n=128)
            for h in range(MH):
                P.mm(stb[:, h * 128:(h + 1) * 128], KTP[:, h, cols], QTP[:, h, cols])
            for h in range(MH):
                P.transpose(ktb_bf[:, h, :], KTP[:, h, cols], IDENTB[:])
            P.tt(STMs[c % 2][:], stb.rearrange("p (h n) -> p h n", n=128),
                 MASKU[:].unsqueeze(1).to_broadcast([128, MH, 128]), ALU.mult)
            for h in range(MH):
                o_, i_, sc_ = KTOKs[c % 2][:, h, :], ktb_bf[:, h, :], EG[:, h, c: c + 1]
                P.add("act", (lambda e, o_=o_, i_=i_, sc_=sc_: e.mul(o_, i_, sc_)), reads=[i_, sc_], writes=[o_])

        rec_pre(0)
        for c in range(NT):
            cols = slice(c * 128, (c + 1) * 128)
            stm, ktok = STMs[c % 2], KTOKs[c % 2]
            ta, tb = TA[c % 2], TB[c % 2]
            if c + 1 < NT:
                rec_pre(c + 1)
            if c < NT - 1:
                for h in range(MH):
                    P.mm(dcb[:, h, 0:128], ktok[:, h, :], VM[:, c, h * 128:(h + 1) * 128])
                    P.mm(dcb[:, h, 128:256], ktok[:, h, :], ONESB[:, 0:128])
            for h in range(MH):
                P.mm(nb[:, h * 128:(h + 1) * 128], VM[:, c, h * 128:(h + 1) * 128], stm[:, h, :], start=True, stop=False)
                P.mm(nb[:, h * 128:(h + 1) * 128], CB[:, h, 0:128], QTP[:, h, cols], start=False, stop=True)
                P.mm(db[:, h * 128:(h + 1) * 128], ONESB[:, 0:128], stm[:, h, :], start=True, stop=False)
                P.mm(db[:, h * 128:(h + 1) * 128], CB[:, h, 128:256], QTP[:, h, cols], start=False, stop=True)
            if c < NT - 1:
                for h in range(MH):
                    P.stt(C32[:, h, :], C32[:, h, :], EG[:, h, c: c + 1], dcb[:, h, :], ALU.mult, ALU.add)
                P.copy(CB[:], C32[:], eng="act")
            P.act(ta[:], db, AF.Abs)
            P.ts(ta[:], ta[:], 1.0, None, ALU.max)
            P.act(ta[:], ta[:], AF.Ln)
            P.act(ta[:], ta[:], AF.Exp, scale=-1.0)
            P.tt(tb[:], nb, ta[:], ALU.mult)
            P.tt(SO[:, :, cols], tb[:].rearrange("p (h n) -> p h n", n=128), SO[:, :, cols], ALU.mult)
        dump("HM", SO)
        dump("R_QTP", QTP); dump("R_KTP", KTP); dump("R_VM", VM); dump("R_C32", C32); dump("R_CB", CB); dump("R_HM", SO)
        for c in range(KC):
            S_.ensure(idx[("wmg", c)])
            S_.prefetch_after(idx[("wmg", c)], idx[("wmg", KC - 1)])
            w = WMG[c % 2]
            for st in range(4):
                cols = slice(st * 512, (st + 1) * 512)
                b0 = 4 * ((c * 4 + st) % 2)
                ab, bb, gab, gbb = bank(b0), bank(b0 + 1), bank(b0 + 2), bank(b0 + 3)
                for kc in range(4):
                    P.mm(ab, w["uf"][:, kc, :], AOT[:, kc, cols], start=(kc == 0), stop=(kc == 3))
                for kc in range(4):
                    P.mm(bb, w["um"][:, kc, :], SO[:, kc, cols], start=(kc == 0), stop=(kc == 3))
                for kc in range(KC):
                    P.mm(gab, w["ga"][:, kc, :], XNT[:, kc, cols], start=(kc == 0), stop=(kc == KC - 1))
                for kc in range(KC):
                    P.mm(gbb, w["gb"][:, kc, :], XNT[:, kc, cols], start=(kc == 0), stop=(kc == KC - 1))
                P.act(SGA[:], gab, AF.Sigmoid)
                P.act(SGB[:], gbb, AF.Sigmoid)
                P.tt(M1[:], SGA[:], ab, ALU.mult)
                P.tt(M2[:], SGB[:], bb, ALU.mult)
                P.tt(MT[:, c, cols], M1[:], M2[:], ALU.add)
        dump("MT", MT)
        S_.ensure(idx["wo"])
        for t in range(NT):
            for dh in range(2):
                yb = bank((t * 2 + dh) % 2)
                for c in range(KC):
                    P.mm(yb, MT[:, c, t * 128:(t + 1) * 128], WO[:, c, dh * 512:(dh + 1) * 512],
                         start=(c == 0), stop=(c == KC - 1))
                xsl = X[:, t, dh * 512:(dh + 1) * 512]
                P.tt(xsl, yb, xsl, ALU.add)
        dump("L_AOT", AOT); dump("L_HM", SO); dump("L_MT", MT); dump("L_EG", EG); dump("L_XNT", XNT)

    x_v = x_d.rearrange("(s t p) d -> s p t d", p=128, t=NT)
    out_v = out_d.rearrange("(s t p) d -> s p t d", p=128, t=NT)

    sched = []
    for s in range(n_seq):
        if do_ffn1:
            sched.append(("ffn", s, 0, ffn_prepare(w1i_d, w1o_d)))
        if do_mix:
            sched.append(("mix", s, 1, mix_prepare()))
        if do_ffn2:
            sched.append(("ffn", s, 2, ffn_prepare(w2i_d, w2o_d)))
        sched.append(("fin", s, None, None))

    fused_final = do_ffn2
    prefetched = set()
    for (kind, s, gidx, idx) in sched:
        first_stage = (kind == "ffn" and gidx == 0) or (kind == "mix" and not do_ffn1) or \
                      (kind == "ffn" and gidx == 2 and not do_ffn1 and not do_mix) or \
                      (kind == "fin" and not (do_ffn1 or do_mix or do_ffn2))
        if first_stage:
            for t in range(NT):
                if (s, t) not in prefetched:
                    P.dma(X[:, t, :], x_v[s, :, t, :], "xinA" if t < 8 else "xinB")
        if kind == "ffn":
            if gidx == 2 and fused_final:
                nxt = s + 1 if s + 1 < n_seq else None
                ffn_phase(s, gidx, idx, final=True, nxt=nxt)
                if nxt is not None:
                    prefetched.update((nxt, t) for t in range(8))
            else:
                ffn_phase(s, gidx, idx)
        elif kind == "mix":
            mixer_phase(s, idx)
        elif kind == "fin" and not fused_final:
            P.dma(GF[:], gfin_d[0:1, :].partition_broadcast(128), "c0")
            rms_rstd(list(range(NT)), 16)
            for t in range(NT):
                ob = XS[t % 2]
                P.stt(ob[:], X[:, t, :], STAT[:, 16 + t: 17 + t], GF[:], ALU.mult, ALU.mult)
                P.dma(out_v[s, :, t, :], ob[:], "xout")
    P.emit()
    return nc


N_CORES = 8


def _prep_shared(inp):
    f = lambda a: np.ascontiguousarray(np.asarray(a, dtype=np.float32))
    g = np.stack([f(inp["ffn1_norm"])[0], f(inp["mix_norm"])[0], f(inp["ffn2_norm"])[0]], 0)
    gfm = np.ascontiguousarray(g.reshape(3, KC, 128).transpose(2, 0, 1).reshape(128, 3 * KC))
    cw = f(inp["mlstm_conv_w"])[0]
    cb = f(inp["mlstm_conv_b"])[0]
    cv = np.concatenate([cw, cb[None, :]], 0)
    conv_fm = np.ascontiguousarray(cv.reshape(5, KC, 128).transpose(2, 1, 0).reshape(128, KC * 5))
    biases = np.concatenate([f(inp["fox_f_bias"])[0], f(inp["mlstm_i_bias"])[0], f(inp["mlstm_f_bias"])[0]])[None, :]
    return {
        "ffn1_w_in": f(inp["ffn1_w_in"])[0], "ffn1_w_out": f(inp["ffn1_w_out"])[0],
        "ffn2_w_in": f(inp["ffn2_w_in"])[0], "ffn2_w_out": f(inp["ffn2_w_out"])[0],
        "w_in": f(inp["w_in"])[0], "w_up_fox": f(inp["w_up_fox"])[0], "w_up_mlstm": f(inp["w_up_mlstm"])[0],
        "w_out": f(inp["w_out"])[0], "gains_fm": gfm, "final_norm": f(inp["final_norm"]).reshape(1, D),
        "conv_fm": conv_fm, "biases": np.ascontiguousarray(biases),
        "fbias_col": np.ascontiguousarray(f(inp["fox_f_bias"])[0].reshape(8, 1)),
    }


_NC_CACHE = {}


def kernel(**inputs):
    x = np.asarray(inputs["x"], dtype=np.float32)
    B = x.shape[0]
    n_seq = B // N_CORES
    shared = _prep_shared(inputs)
    key = n_seq
    if key not in _NC_CACHE:
        _NC_CACHE[key] = build_program(n_seq)
    nc = _NC_CACHE[key]
    in_maps = []
    for c in range(N_CORES):
        m = dict(shared)
        m["x"] = np.ascontiguousarray(x[c * n_seq:(c + 1) * n_seq].reshape(n_seq * SEQ, D))
        in_maps.append(m)
    res = run_bass_kernel_spmd(nc, in_maps, core_ids=list(range(N_CORES)))
    out = np.concatenate([np.asarray(r["out"]).reshape(n_seq, SEQ, D) for r in res.results], 0)
    return out.astype(np.float32)
```
